# Optimizing a Trainium2 kernel written in Bass

```python
import jax, jax.numpy as jnp
from jax import lax
import numpy as np

D_MODEL = 2048
BATCH = 2
SEQ = 4096
DEPTH = 2

HEAD_DIM = 64
A_HEADS = 12
A_PATTERNS = ((128, 1), (512, 4), (2048, 16))
B_HEADS = 12
B_KV_HEADS = 3
B_BRANCHES = 3
CMP_BLOCK = 32
CMP_STRIDE = 16
CMP_HIDDEN = 128
SLC_BLOCK = 64
SLC_TOPK = 16
WIN = 512
C_HEADS = 8
N_MIX_HEADS = A_HEADS + B_HEADS + C_HEADS
MIX_WIDTH = N_MIX_HEADS * HEAD_DIM
A_QKV_W = 3 * A_HEADS * HEAD_DIM
B_Q_W = B_HEADS * HEAD_DIM
B_KV_W = 2 * B_BRANCHES * B_KV_HEADS * HEAD_DIM
B_GATE_W = B_BRANCHES * B_HEADS
C_QKV_W = 3 * C_HEADS * HEAD_DIM
IN_WIDTH = A_QKV_W + B_Q_W + B_KV_W + B_GATE_W + C_QKV_W
IN_SPLITS = (A_QKV_W, A_QKV_W + B_Q_W, A_QKV_W + B_Q_W + B_KV_W, A_QKV_W + B_Q_W + B_KV_W + B_GATE_W)
N_EXPERTS = 32
TOP_K = 4
D_FF = 2048
SWIGLU_LIMIT = 7.0
SWIGLU_ALPHA = 1.702
ROPE_THETA = 10000.0
EPS = 1e-6
Q_BLOCK = 128
MOE_BLOCK = 128
NEG_BIG = -1e30
TINY = 1e-30
FORCE = 1e4

kernel_name = 'hybrid_dilated_nsa_stickbreak_moe_block'

F32 = jnp.float32


def rms_norm(x, g):
    xf = x.astype(F32)
    y = xf * lax.rsqrt(jnp.mean(xf * xf, axis=-1, keepdims=True) + EPS)
    return (y * g.astype(F32)).astype(x.dtype)


def rope_tables(seq):
    inv = 1.0 / (ROPE_THETA ** (jnp.arange(0, HEAD_DIM, 2, dtype=F32) / HEAD_DIM))
    ang = jnp.arange(seq, dtype=F32)[:, None] * inv[None, :]
    return jnp.cos(ang), jnp.sin(ang)


def apply_rope(x, cos, sin):
    x1, x2 = jnp.split(x.astype(F32), 2, axis=-1)
    return jnp.concatenate([x1 * cos - x2 * sin, x2 * cos + x1 * sin], axis=-1).astype(x.dtype)


def to_heads(t, n):
    B, T, _ = t.shape
    return t.reshape(B, T, n, HEAD_DIM).transpose(0, 2, 1, 3)


def attend(s, valid, v, spec):
    s = jnp.where(valid, s, NEG_BIG)
    m = jnp.max(s, axis=-1, keepdims=True)
    p = jnp.exp(s - m) * valid
    p = p / jnp.maximum(jnp.sum(p, axis=-1, keepdims=True), TINY)
    return jnp.einsum(spec, p, v), p


def dilated_attention(q, k, v):
    B, H, T, dh = q.shape
    scale = dh ** -0.5
    kf, vf = k.astype(F32), v.astype(F32)

    def block(i):
        q0 = i * Q_BLOCK
        t = q0 + jnp.arange(Q_BLOCK)
        qb = lax.dynamic_slice_in_dim(q, q0, Q_BLOCK, axis=2).astype(F32)
        outs, maxes, dens = [], [], []
        for window, dil in A_PATTERNS:
            offs = jnp.arange(window // dil + 1) * dil
            idx = t[:, None] - offs[None, :]
            valid = idx >= 0
            idx = jnp.maximum(idx, 0)
            kg, vg = kf[:, :, idx], vf[:, :, idx]
            s = jnp.einsum('bhqd,bhqnd->bhqn', qb, kg) * scale
            s = jnp.where(valid, s, NEG_BIG)
            m = jnp.max(s, axis=-1, keepdims=True)
            p = jnp.exp(s - m)
            l = jnp.sum(p, axis=-1, keepdims=True)
            outs.append(jnp.einsum('bhqn,bhqnd->bhqd', p, vg) / l)
            maxes.append(m)
            dens.append(l)
        m_all = jnp.max(jnp.stack(maxes), axis=0)
        w = jnp.stack([l * jnp.exp(m - m_all) for m, l in zip(maxes, dens)])
        o = jnp.sum(w * jnp.stack(outs), axis=0) / jnp.sum(w, axis=0)
        return o.astype(q.dtype)

    out = lax.map(block, jnp.arange(T // Q_BLOCK))
    return out.transpose(1, 2, 0, 3, 4).reshape(B, H, T, dh)


def compress(xk, pe, w1, w2):
    T = xk.shape[2]
    n_cmp = (T - CMP_BLOCK) // CMP_STRIDE + 1
    idx = np.arange(n_cmp)[:, None] * CMP_STRIDE + np.arange(CMP_BLOCK)[None, :]
    blocks = xk[:, :, idx] + pe
    flat = blocks.reshape(blocks.shape[0], blocks.shape[1], n_cmp, CMP_BLOCK * HEAD_DIM)
    return jax.nn.gelu(flat @ w1) @ w2


def cmp_to_slc_matrix(T):
    n_cmp = (T - CMP_BLOCK) // CMP_STRIDE + 1
    n_slc = T // SLC_BLOCK
    cs = np.arange(n_cmp) * CMP_STRIDE
    ss = np.arange(n_slc) * SLC_BLOCK
    ov = (cs[:, None] < ss[None, :] + SLC_BLOCK) & (cs[:, None] + CMP_BLOCK > ss[None, :])
    return jnp.asarray(ov.astype(np.float32))


def nsa_attention(q, kc, vc, ks, vs, kw, vw, gates):
    B, Hq, T, dh = q.shape
    G = ks.shape[1]
    R = Hq // G
    n_cmp = kc.shape[2]
    n_slc = T // SLC_BLOCK
    top = min(SLC_TOPK, n_slc)
    scale = dh ** -0.5
    cmp_end = jnp.arange(n_cmp) * CMP_STRIDE + (CMP_BLOCK - 1)
    overlap = cmp_to_slc_matrix(T)
    slc_id = jnp.arange(n_slc)
    kcf, vcf = kc.astype(F32), vc.astype(F32)
    ksb = ks.reshape(B, G, n_slc, SLC_BLOCK, dh)
    vsb = vs.reshape(B, G, n_slc, SLC_BLOCK, dh)
    kwp = jnp.pad(kw, ((0, 0), (0, 0), (WIN, 0), (0, 0)))
    vwp = jnp.pad(vw, ((0, 0), (0, 0), (WIN, 0), (0, 0)))
    b_ix = jnp.arange(B)[:, None, None, None]
    g_ix = jnp.arange(G)[None, :, None, None]

    def block(i):
        q0 = i * Q_BLOCK
        t = q0 + jnp.arange(Q_BLOCK)
        qg = lax.dynamic_slice_in_dim(q, q0, Q_BLOCK, axis=2).astype(F32).reshape(B, G, R, Q_BLOCK, dh)
        s = jnp.einsum('bgrqd,bgnd->bgrqn', qg, kcf) * scale
        o_cmp, p_cmp = attend(s, cmp_end[None, :] <= t[:, None], vcf, 'bgrqn,bgnd->bgrqd')
        imp = jnp.einsum('bgrqn,nm->bgqm', p_cmp, overlap)
        tb = t // SLC_BLOCK
        forced = (slc_id[None, :] == 0) | (slc_id[None, :] == tb[:, None]) | (slc_id[None, :] == tb[:, None] - 1)
        imp = jnp.where(forced, imp + FORCE, imp)
        imp = jnp.where(slc_id[None, :] > tb[:, None], -FORCE, imp)
        _, sel = lax.top_k(imp, top)
        kg = ksb[b_ix, g_ix, sel].reshape(B, G, Q_BLOCK, top * SLC_BLOCK, dh).astype(F32)
        vg = vsb[b_ix, g_ix, sel].reshape(B, G, Q_BLOCK, top * SLC_BLOCK, dh).astype(F32)
        pos = (sel[..., None] * SLC_BLOCK + jnp.arange(SLC_BLOCK)).reshape(B, G, Q_BLOCK, top * SLC_BLOCK)
        valid = (pos <= t[None, None, :, None])[:, :, None]
        s = jnp.einsum('bgrqd,bgqkd->bgrqk', qg, kg) * scale
        o_slc, _ = attend(s, valid, vg, 'bgrqk,bgqkd->bgrqd')
        kwb = lax.dynamic_slice_in_dim(kwp, q0, Q_BLOCK + WIN, axis=2).astype(F32)
        vwb = lax.dynamic_slice_in_dim(vwp, q0, Q_BLOCK + WIN, axis=2).astype(F32)
        kp = q0 - WIN + jnp.arange(Q_BLOCK + WIN)
        valid = (kp[None, :] <= t[:, None]) & (kp[None, :] > t[:, None] - WIN) & (kp[None, :] >= 0)
        s = jnp.einsum('bgrqd,bgkd->bgrqk', qg, kwb) * scale
        o_win, _ = attend(s, valid, vwb, 'bgrqk,bgkd->bgrqd')
        gb = lax.dynamic_slice_in_dim(gates, q0, Q_BLOCK, axis=2).reshape(B, G, R, Q_BLOCK, B_BRANCHES)
        o = gb[..., 0:1] * o_cmp + gb[..., 1:2] * o_slc + gb[..., 2:3] * o_win
        return o.reshape(B, Hq, Q_BLOCK, dh).astype(q.dtype)

    out = lax.map(block, jnp.arange(T // Q_BLOCK))
    return out.transpose(1, 2, 0, 3, 4).reshape(B, Hq, T, dh)


def stick_breaking_attention(q, k, v):
    B, H, T, dh = q.shape
    scale = dh ** -0.5
    kf, vf = k.astype(F32), v.astype(F32)
    key_pos = jnp.arange(T)

    def block(i):
        q0 = i * Q_BLOCK
        t = q0 + jnp.arange(Q_BLOCK)
        qb = lax.dynamic_slice_in_dim(q, q0, Q_BLOCK, axis=2).astype(F32)
        z = jnp.einsum('bhqd,bhkd->bhqk', qb, kf) * scale
        before = key_pos[None, :] < t[:, None]
        log_1m = jnp.where(before, jax.nn.log_sigmoid(-z), 0.0)
        suffix = lax.cumsum(log_1m, axis=3, reverse=True) - log_1m
        a = jnp.where(before, jnp.exp(jax.nn.log_sigmoid(z) + suffix), 0.0)
        return jnp.einsum('bhqk,bhkd->bhqd', a, vf).astype(q.dtype)

    out = lax.map(block, jnp.arange(T // Q_BLOCK))
    return out.transpose(1, 2, 0, 3, 4).reshape(B, H, T, dh)


def hybrid_mixer(h, w_in, cmp_pe, cmp_w1, cmp_w2, mix_norm, w_out, cos, sin):
    B, T, _ = h.shape
    proj = h @ w_in
    a_qkv, b_q, b_kv, b_gate, c_qkv = jnp.split(proj, IN_SPLITS, axis=-1)
    aq, ak, av = [to_heads(u, A_HEADS) for u in jnp.split(a_qkv, 3, axis=-1)]
    o_a = dilated_attention(apply_rope(aq, cos, sin), apply_rope(ak, cos, sin), av)
    bq = apply_rope(to_heads(b_q, B_HEADS), cos, sin)
    k_c, v_c, k_s, v_s, k_w, v_w = [to_heads(u, B_KV_HEADS) for u in jnp.split(b_kv, 6, axis=-1)]
    n_cmp = (T - CMP_BLOCK) // CMP_STRIDE + 1
    cmp_end = np.arange(n_cmp) * CMP_STRIDE + CMP_BLOCK - 1
    kc = apply_rope(compress(k_c, cmp_pe[0], cmp_w1[0], cmp_w2[0]), cos[cmp_end], sin[cmp_end])
    vc = compress(v_c, cmp_pe[1], cmp_w1[1], cmp_w2[1])
    gates = jax.nn.sigmoid(b_gate.reshape(B, T, B_HEADS, B_BRANCHES).astype(F32)).transpose(0, 2, 1, 3)
    o_b = nsa_attention(bq, kc, vc, apply_rope(k_s, cos, sin), v_s, apply_rope(k_w, cos, sin), v_w, gates)
    cq, ck, cv = [to_heads(u, C_HEADS) for u in jnp.split(c_qkv, 3, axis=-1)]
    o_c = stick_breaking_attention(cq, ck, cv)
    o = jnp.concatenate([o_a, o_b, o_c], axis=1).transpose(0, 2, 1, 3)
    of = o.astype(F32)
    of = of * lax.rsqrt(jnp.mean(of * of, axis=-1, keepdims=True) + EPS)
    o = (of.reshape(B, T, MIX_WIDTH) * mix_norm.astype(F32)).astype(h.dtype)
    return o @ w_out


def moe_ffn(h, w_router, b_router, w1, b1, w2, b2):
    B, T, D = h.shape
    N = B * T
    xt = h.reshape(N, D)
    logits = (xt @ w_router + b_router).astype(F32)
    top_val, top_idx = lax.top_k(logits, TOP_K)
    gate = jax.nn.softmax(top_val, axis=-1)
    NK = N * TOP_K
    e_flat = top_idx.reshape(NK)
    tok_flat = jnp.repeat(jnp.arange(N, dtype=jnp.int32), TOP_K)
    w_flat = gate.reshape(NK)
    order = jnp.argsort(e_flat)
    e_sorted, tok_sorted, w_sorted = e_flat[order], tok_flat[order], w_flat[order]
    counts = jnp.bincount(e_flat, length=N_EXPERTS)
    padded = (counts + MOE_BLOCK - 1) // MOE_BLOCK * MOE_BLOCK
    pad_end = jnp.cumsum(padded)
    pad_start = pad_end - padded
    grp_start = jnp.cumsum(counts) - counts
    dest = pad_start[e_sorted] + jnp.arange(NK) - grp_start[e_sorted]
    P = NK + N_EXPERTS * MOE_BLOCK
    n_blk = P // MOE_BLOCK
    buf_tok = jnp.zeros((P,), jnp.int32).at[dest].set(tok_sorted)
    buf_w = jnp.zeros((P,), F32).at[dest].set(w_sorted)
    blk_start = jnp.arange(n_blk) * MOE_BLOCK
    blk_expert = jnp.minimum(jnp.sum(blk_start[:, None] >= pad_end[None, :], axis=-1), N_EXPERTS - 1)

    def block(args):
        tok, e = args
        hc = xt[tok] @ w1[e] + b1[e]
        glu, lin = jnp.split(hc, 2, axis=-1)
        glu = jnp.minimum(glu, SWIGLU_LIMIT)
        lin = jnp.clip(lin, -SWIGLU_LIMIT, SWIGLU_LIMIT)
        act = glu * jax.nn.sigmoid(SWIGLU_ALPHA * glu) * (lin + 1)
        return act @ w2[e] + b2[e]

    y = lax.map(block, (buf_tok.reshape(n_blk, MOE_BLOCK), blk_expert)).reshape(P, D)
    y = y * buf_w[:, None].astype(y.dtype)
    out = jnp.zeros((N, D), y.dtype).at[buf_tok].add(y)
    return out.reshape(B, T, D)


def setup_inputs(seed: int = 0) -> dict:
    key = jax.random.key(seed)
    ks = jax.random.split(key, 19)
    L, D, E, F = DEPTH, D_MODEL, N_EXPERTS, D_FF

    def nrm(k, shape, scale):
        return scale * jax.random.normal(k, shape, F32)

    return {
        'x': nrm(ks[0], (BATCH, SEQ, D), 1.0),
        'c': nrm(ks[1], (BATCH, D), 1.0),
        'w_mod': nrm(ks[2], (L, D, 6 * D), 0.5 * D ** -0.5),
        'b_mod': nrm(ks[3], (L, 6 * D), 0.02),
        'norm_attn': 1.0 + nrm(ks[4], (L, D), 0.05),
        'norm_ffn': 1.0 + nrm(ks[5], (L, D), 0.05),
        'w_in': nrm(ks[6], (L, D, IN_WIDTH), D ** -0.5),
        'cmp_pe': nrm(ks[7], (L, 2, CMP_BLOCK, HEAD_DIM), 0.1),
        'cmp_w1': nrm(ks[8], (L, 2, CMP_BLOCK * HEAD_DIM, CMP_HIDDEN), (CMP_BLOCK * HEAD_DIM) ** -0.5),
        'cmp_w2': nrm(ks[9], (L, 2, CMP_HIDDEN, HEAD_DIM), CMP_HIDDEN ** -0.5),
        'mix_norm': 1.0 + nrm(ks[10], (L, MIX_WIDTH), 0.05),
        'w_out': nrm(ks[11], (L, MIX_WIDTH, D), MIX_WIDTH ** -0.5),
        'w_router': nrm(ks[12], (L, D, E), D ** -0.5),
        'b_router': nrm(ks[13], (L, E), 0.01),
        'w_exp1': nrm(ks[14], (L, E, D, 2 * F), D ** -0.5),
        'b_exp1': nrm(ks[15], (L, E, 2 * F), 0.02),
        'w_exp2': nrm(ks[16], (L, E, F, D), F ** -0.5),
        'b_exp2': nrm(ks[17], (L, E, D), 0.02),
        'norm_final': 1.0 + nrm(ks[18], (D,), 0.05),
    }


def reference(x, c, w_mod, b_mod, norm_attn, norm_ffn, w_in, cmp_pe, cmp_w1, cmp_w2,
              mix_norm, w_out, w_router, b_router, w_exp1, b_exp1, w_exp2, b_exp2, norm_final):
    T = x.shape[1]
    cos, sin = rope_tables(T)
    c_act = jax.nn.silu(c)
    for i in range(DEPTH):
        mod = c_act @ w_mod[i] + b_mod[i]
        sh1, sc1, g1, sh2, sc2, g2 = [m[:, None, :] for m in jnp.split(mod, 6, axis=-1)]
        h = rms_norm(x, norm_attn[i]) * (1 + sc1) + sh1
        x = x + g1 * hybrid_mixer(h, w_in[i], cmp_pe[i], cmp_w1[i], cmp_w2[i], mix_norm[i], w_out[i], cos, sin)
        h = rms_norm(x, norm_ffn[i]) * (1 + sc2) + sh2
        x = x + g2 * moe_ffn(h, w_router[i], b_router[i], w_exp1[i], b_exp1[i], w_exp2[i], b_exp2[i])
    return rms_norm(x, norm_final)
```

```python
import contextlib
import numpy as np
import ml_dtypes
import concourse.bass as bass
import concourse.mybir as mybir
from concourse.bass_utils import run_bass_kernel_spmd

F32 = mybir.dt.float32
BF16 = mybir.dt.bfloat16
ALU = mybir.AluOpType
AF = mybir.ActivationFunctionType
AX = mybir.AxisListType
NPBF = ml_dtypes.bfloat16

D = 2048
TL = 4096
NQ = 1024
QOFF = TL - NQ
NT = TL // 128
NTOK = TL + NQ
INW = 5796
CAP = 512
NE = 32
DFF = 2048
EPS = 1e-6
SCALE = 0.125
NDS = 40


class Buf:
    __slots__ = ("name", "w", "r")

    def __init__(self, name=""):
        self.name = name
        self.w = None
        self.r = {}


class Tl:
    __slots__ = ("t", "b")

    def __init__(self, t, b):
        self.t = t
        self.b = b


class KB:
    def __init__(self, nc, es):
        self.nc = nc
        self.es = es
        self.engs = {"pe": nc.tensor, "act": nc.scalar, "dve": nc.vector,
                     "pool": nc.gpsimd, "sp": nc.sync}
        self.csem = {e: es.enter_context(nc.semaphore("c_" + e)) for e in ("pe", "act", "dve", "pool")}
        self.ccnt = {e: 0 for e in self.csem}
        self.dsems = [es.enter_context(nc.semaphore("dq%d" % i)) for i in range(NDS)]
        self.dcnt = [0] * NDS
        self.dnext = 0
        self.known = {e: {} for e in self.engs}
        self.n = 0

    @contextlib.contextmanager
    def scope(self):
        old = self.es
        with contextlib.ExitStack() as es:
            self.es = es
            try:
                yield
            finally:
                self.barrier()
                self.es = old

    def sb(self, name, shape, dt):
        self.n += 1
        t = self.es.enter_context(self.nc.sbuf_tensor("%s_%d" % (name, self.n), list(shape), dt))
        return Tl(t, Buf(name))

    def ps(self, name, shape, dt=F32):
        self.n += 1
        t = self.es.enter_context(self.nc.psum_tensor("%s_%d" % (name, self.n), list(shape), dt))
        return Tl(t, Buf(name))

    def _wait(self, eng, tok):
        key, sem, val = tok
        if self.known[eng].get(key, 0) >= val:
            return
        self.engs[eng].wait_ge(sem, val)
        self.known[eng][key] = val

    def _deps(self, eng, reads, writes):
        toks = []
        for b in reads:
            if b.w is not None:
                toks.append(b.w)
        for b in writes:
            if b.w is not None and b.w[0] != eng:
                toks.append(b.w)
            for kk, t in b.r.items():
                if kk != eng:
                    toks.append(t)
        for t in toks:
            if eng == "pe" and t[0] == "pe":
                continue
            self._wait(eng, t)

    def _mark(self, tok, reads, writes):
        for b in reads:
            old = b.r.get(tok[0])
            if old is None or old[2] < tok[2]:
                b.r[tok[0]] = tok
        for b in writes:
            b.w = tok
            b.r = {}

    def op(self, eng, fn, reads=(), writes=()):
        reads = [x.b if isinstance(x, Tl) else x for x in reads]
        writes = [x.b if isinstance(x, Tl) else x for x in writes]
        self._deps(eng, reads, writes)
        ins = fn(self.engs[eng])
        self.ccnt[eng] += 1
        ins.then_inc(self.csem[eng], 1)
        tok = (eng, self.csem[eng], self.ccnt[eng])
        self._mark(tok, reads, writes)
        return tok

    def dma(self, q, pairs, reads=(), writes=(), **kw):
        reads = [x.b if isinstance(x, Tl) else x for x in reads]
        writes = [x.b if isinstance(x, Tl) else x for x in writes]
        self._deps(q, reads, writes)
        i = self.dnext
        self.dnext = (self.dnext + 1) % NDS
        sem = self.dsems[i]
        key = "d%d" % i
        if self.dcnt[i] > 0:
            self._wait(q, (key, sem, self.dcnt[i]))
        for (o, a) in pairs:
            self.engs[q].dma_start(out=o, in_=a, **kw).then_inc(sem, 16)
            self.dcnt[i] += 16
        tok = (key, sem, self.dcnt[i])
        self._mark(tok, reads, writes)
        return tok

    def _alltoks(self):
        toks = [(e, self.csem[e], self.ccnt[e]) for e in self.csem if self.ccnt[e] > 0]
        toks += [("d%d" % i, self.dsems[i], self.dcnt[i]) for i in range(NDS) if self.dcnt[i] > 0]
        return toks

    def barrier(self):
        toks = self._alltoks()
        for e in self.engs:
            for t in toks:
                if t[0] != e:
                    self._wait(e, t)

    def finish(self, eng="sp"):
        for t in self._alltoks():
            self._wait(eng, t)


def _rope_rows(pos, nrows):
    inv = (1.0 / (10000.0 ** (np.arange(0, 64, 2, dtype=np.float32) / np.float32(64)))).astype(np.float32)
    ang = pos.astype(np.float32)[None, :] * inv[:, None]
    cos, sin = np.cos(ang).astype(np.float32), np.sin(ang).astype(np.float32)
    p = np.arange(nrows)
    c2 = cos[p % 32]
    sgn = np.where((p % 64) < 32, -1.0, 1.0).astype(np.float32)
    s2 = sin[p % 32] * sgn[:, None]
    return np.stack([c2, s2]).astype(np.float32)


def shared_consts():
    c = {}
    q = np.arange(512)[None, :]
    s = np.arange(128)[:, None]
    mA = np.zeros((20, 128, 512), np.float32)
    for idx in range(20):
        d = 128 * (idx - 3) + (q - s)
        m = ((d >= 0) & (d <= 128)).astype(np.float32)
        m += ((d >= 0) & (d <= 512) & (d % 4 == 0))
        m += ((d >= 0) & (d <= 2048) & (d % 16 == 0))
        mA[idx] = m
    c["maskA"] = mA.astype(NPBF)
    mC = np.zeros((4, 128, 512), np.float32)
    mS = np.zeros((4, 128, 512), np.float32)
    for dl in range(4):
        sa = 128 * dl + s
        mC[dl] = (sa <= q)
        mS[dl] = (sa < q)
    c["maskCaus"] = mC.astype(NPBF)
    c["maskStrictF"] = mS.astype(np.float32)
    c["maskStrictB"] = mS.astype(NPBF)
    mW = np.zeros((8, 128, 512), np.float32)
    for i in range(8):
        d = q - (128 * (i - 4) + s)
        mW[i] = (d >= 0) & (d < 512)
    c["maskW"] = mW.astype(NPBF)
    mCmp = np.zeros((2, 2, 128, 512), np.float32)
    for qg in range(2):
        for nt in range(2):
            n = 128 * nt + s
            t = QOFF + 512 * qg + q
            mCmp[qg, nt] = (16 * n + 31 <= t) & (n <= 254)
    c["maskCmp"] = mCmp.astype(NPBF)
    ss = np.arange(TL)
    c["expand"] = (ss[None, :] // 64 == np.arange(64)[:, None]).astype(NPBF)
    j = np.arange(128)
    c["u2"] = (j[:, None] > j[None, :]).astype(np.float32)
    c["onesf"] = np.ones((128, 128), np.float32)
    c["tri"] = (j[:, None] < j[None, :]).astype(NPBF)
    c["onesb"] = np.ones((128, 128), NPBF)
    c["identf"] = np.eye(128, dtype=np.float32)
    c["identb"] = np.eye(128).astype(NPBF)
    sw = np.zeros((128, 128), np.float32)
    for m in range(128):
        sw[(m + 32) % 64 + 64 * (m // 64), m] = 1.0
    c["pswap"] = sw
    c["iota"] = np.tile(np.arange(CAP, dtype=np.float32)[None, :], (128, 1))
    return c


def core_consts(r):
    pad = 1024 * (3 - r)
    c = {}
    posl = np.arange(TL) - pad
    pos_all = np.concatenate([posl, posl[QOFF:]])
    c["rope"] = _rope_rows(pos_all, 128)
    n = np.arange(256)
    c["ropec"] = _rope_rows(16 * n + 31 - pad, 64)
    tl = np.arange(TL).reshape(NT, 128).T
    c["kvalid"] = (tl >= pad).astype(np.float32)
    nn = np.arange(256).reshape(2, 128).T
    cv = ((nn <= 254) & (16 * nn >= pad)).astype(np.float32)
    c["cvalid"] = cv
    m = np.arange(64)
    cs = 16 * np.arange(256)[:, None]
    sm = 64 * m[None, :]
    ov = ((cs < sm + 64) & (cs + 32 > sm)).astype(np.float32)
    ov = ov.reshape(2, 128, 64).transpose(1, 0, 2) * cv[:, :, None]
    c["ovl"] = ov.astype(NPBF)
    t = QOFF + np.arange(NQ)[:, None]
    tb = t // 64
    m0 = pad // 64
    mm = m[None, :]
    bias = np.zeros((NQ, 64), np.float32)
    forced = (mm == m0) | (mm == tb) | (mm == tb - 1)
    bias[forced] = 1e4
    bias[np.broadcast_to(mm > tb, bias.shape)] = -1e4
    bias[np.broadcast_to(mm < m0, bias.shape)] = -1e4
    c["impbias"] = bias
    return c


CONST_SPECS = {
    "maskA": ([20, 128, 512], BF16), "maskCaus": ([4, 128, 512], BF16),
    "maskStrictF": ([4, 128, 512], F32), "maskStrictB": ([4, 128, 512], BF16),
    "maskW": ([8, 128, 512], BF16), "maskCmp": ([2, 2, 128, 512], BF16),
    "expand": ([64, TL], BF16), "u2": ([128, 128], F32), "onesf": ([128, 128], F32),
    "tri": ([128, 128], BF16), "onesb": ([128, 128], BF16), "identf": ([128, 128], F32),
    "identb": ([128, 128], BF16), "pswap": ([128, 128], F32), "iota": ([128, CAP], F32),
    "rope": ([2, 128, NTOK], F32), "ropec": ([2, 64, 256], F32), "kvalid": ([128, NT], F32),
    "cvalid": ([128, 2], F32), "ovl": ([128, 2, 64], BF16), "impbias": ([NQ, 64], F32),
}

WEIGHT_SPECS = {
    "w_mod": [D, 6 * D], "b_mod": [6 * D], "norm_attn": [D], "norm_ffn": [D],
    "w_in": [D, INW], "cmp_pe": [2, 32, 64], "cmp_w1": [2, 2048, 128], "cmp_w2": [2, 128, 64],
    "mix_norm": [D], "w_out": [D, D], "w_router": [D, NE], "b_router": [NE],
    "w_exp1": [NE, D, 2 * DFF], "b_exp1": [NE, 2 * DFF], "w_exp2": [NE, DFF, D], "b_exp2": [NE, D],
    "norm_final": [D],
}

SCRATCH = {
    "vecs": ([6, D], F32), "kTA": ([768, TL], BF16), "vA": ([TL, 768], BF16), "cT": ([384, TL], BF16),
    "kTS": ([192, TL], BF16), "kTW": ([192, TL], BF16), "vS": ([TL, 192], BF16), "vW": ([TL, 192], BF16),
    "kTC": ([512, TL], BF16), "vC": ([TL, 512], BF16), "qTA": ([768, NQ], BF16), "qTB": ([768, NQ], BF16),
    "qTC": ([512, NQ], BF16), "gates": ([NQ, 36], F32), "otok": ([NQ, D], BF16), "x1": ([NQ, D], F32), "h2s": ([16, 128, 8, 128], BF16),
}


class Prog:
    def __init__(self, last, debug=(), phases=None):
        self.last = last
        self.debug = set(debug)
        self.phases = phases
        nc = bass.Bass("TRN2", target_bir_lowering=False)
        self.nc = nc
        specs = {"xin": ([NTOK, D], F32), "cvec": ([D], F32)}
        specs.update({n: (shp, F32) for n, shp in WEIGHT_SPECS.items()})
        specs.update(CONST_SPECS)

        class Lazy(dict):
            def __missing__(d, n):
                shp, dt = specs[n]
                d[n] = nc.dram_tensor(n, shp, dt, kind="ExternalInput").ap()
                return d[n]
        self.I = Lazy()
        self.S = {}
        for n, (shp, dt) in SCRATCH.items():
            kind = "ExternalOutput" if n in self.debug else "Internal"
            self.S[n] = nc.dram_tensor("s_" + n, shp, dt, kind=kind).ap()
        self.SB = {n: Buf("s_" + n) for n in SCRATCH}
        self.out = nc.dram_tensor("out", [NQ, D], F32, kind="ExternalOutput").ap()
        self.outB = Buf("out")

    def run(self, what):
        return self.phases is None or what in self.phases

    def build(self):
        with contextlib.ExitStack() as es:
            k = KB(self.nc, es)
            self.k = k
            if self.run("mod"):
                with k.scope():
                    self.phase_mod()
            if self.run("proj"):
                with k.scope():
                    self.phase_proj()
            if self.run("attA"):
                with k.scope():
                    self.phase_att_a()
            if self.run("attB"):
                with k.scope():
                    self.phase_att_b()
            if self.run("attC"):
                with k.scope():
                    self.phase_att_c()
            if self.run("oproj"):
                with k.scope():
                    self.phase_oproj()
            if self.run("moe"):
                with k.scope():
                    self.phase_moe()
            k.finish("sp")
        return self.nc

    def load_const(self, name, q="sp", rearr=None, sl=None):
        k = self.k
        shp, dt = CONST_SPECS[name]
        src = self.I[name]
        if rearr is not None:
            src = src.rearrange(rearr)
        t = k.sb(name, list(src.shape), dt)
        k.dma(q, [(t.t[:], src)], writes=[t])
        return t

    def phase_mod(self):
        k, I = self.k, self.I
        cT = k.sb("cT", [128, 16], F32)
        k.dma("sp", [(cT.t[:], I["cvec"].rearrange("(k p) -> p k", p=128))], writes=[cT],
              allow_slow_non_contiguous=True)
        cact = k.sb("cact", [128, 16], F32)
        k.op("act", lambda e: e.activation(cact.t[:], cT.t[:], AF.Silu), reads=[cT], writes=[cact])
        modrow = k.sb("modrow", [1, 6 * D], F32)
        k.dma("sp", [(modrow.t[:], I["b_mod"].rearrange("(o n) -> o n", o=1))], writes=[modrow])
        nrow = k.sb("nrow", [1, 2, D], F32)
        k.dma("sp", [(nrow.t[:, 0, :], I["norm_attn"].rearrange("(o n) -> o n", o=1)),
                     (nrow.t[:, 1, :], I["norm_ffn"].rearrange("(o n) -> o n", o=1))], writes=[nrow])
        wt = [k.sb("wmod%d" % i, [128, 16, 512], F32) for i in range(2)]
        pm = [k.ps("pm%d" % i, [1, 512]) for i in range(2)]
        wsrc = I["w_mod"]
        for n in range(24):
            w = wt[n % 2]
            src = wsrc[:, n * 512:(n + 1) * 512].rearrange("(k p) n -> p k n", p=128)
            k.dma("sp", [(w.t[:, 4 * i:4 * i + 4, :], src[:, 4 * i:4 * i + 4, :]) for i in range(4)], writes=[w])
            p = pm[n % 2]
            for kk in range(16):
                k.op("pe", lambda e, kk=kk, w=w, p=p: e.matmul(p.t[:], cact.t[:, kk:kk + 1], w.t[:, kk, :],
                                                             start=(kk == 0), stop=(kk == 15)),
                     reads=[cact, w], writes=[p])
            k.op("dve", lambda e, n=n, p=p: e.tensor_tensor(modrow.t[:, n * 512:(n + 1) * 512], p.t[:],
                                                          modrow.t[:, n * 512:(n + 1) * 512], ALU.add),
                 reads=[p, modrow], writes=[modrow])
        m = lambda i: modrow.t[:, i * D:(i + 1) * D]
        k.op("dve", lambda e: e.scalar_tensor_tensor(m(1), m(1), 1.0, nrow.t[:, 0, :], ALU.add, ALU.mult),
             reads=[modrow, nrow], writes=[modrow])
        k.op("dve", lambda e: e.scalar_tensor_tensor(m(4), m(4), 1.0, nrow.t[:, 1, :], ALU.add, ALU.mult),
             reads=[modrow, nrow], writes=[modrow])
        order = [1, 0, 2, 4, 3, 5]
        k.dma("sp", [(self.S["vecs"][i:i + 1, :], m(sg)) for i, sg in enumerate(order)], reads=[modrow],
              writes=[self.SB["vecs"]])

    def bvec(self, idx, name, q="sp"):
        k = self.k
        t = k.sb(name, [128, D], F32)
        k.dma(q, [(t.t[:], self.S["vecs"][idx, :].partition_broadcast(128))], reads=[self.SB["vecs"]], writes=[t])
        return t

    def norm_tile(self, xt, gamb, shb, hb, ms, junk, tmp):
        k = self.k
        k.op("pool", lambda e: e.memset(ms.t[:, 0:1], 0.0), writes=[ms])
        k.op("act", lambda e: e.activation(junk.t[:], xt.t[:], AF.Square, scale=float(D ** -0.5),
                                           accum_out=ms.t[:, 0:1]), reads=[xt, ms], writes=[junk, ms])
        k.op("act", lambda e: e.activation(ms.t[:, 1:2], ms.t[:, 0:1], AF.Sqrt, bias=EPS, scale=1.0),
             reads=[ms], writes=[ms])
        k.op("dve", lambda e: e.reciprocal(ms.t[:, 2:3], ms.t[:, 1:2]), reads=[ms], writes=[ms])
        k.op("dve", lambda e: e.scalar_tensor_tensor(tmp.t[:], xt.t[:], ms.t[:, 2:3], gamb.t[:], ALU.mult, ALU.mult),
             reads=[xt, ms, gamb], writes=[tmp])
        k.op("pool", lambda e: e.tensor_tensor(hb.t[:], tmp.t[:], shb.t[:], ALU.add), reads=[tmp, shb], writes=[hb])

    def phase_proj(self):
        k, I, S = self.k, self.I, self.S
        gamb = self.bvec(0, "gam1b")
        shb = self.bvec(1, "sh1b", q="act")
        identb = self.load_const("identb")
        pswap = self.load_const("pswap")
        kval = self.load_const("kvalid")
        xt = [k.sb("xt%d" % i, [128, D], F32) for i in range(2)]
        junk = k.sb("junk", [128, D], BF16)
        tmp = k.sb("tmpn", [128, D], F32)
        hb = [k.sb("hb%d" % i, [128, D], BF16) for i in range(2)]
        ms = [k.sb("ms%d" % i, [128, 4], F32) for i in range(2)]
        hT = k.sb("hT", [128, 16, 1024], BF16)
        hTb = [Buf("hT%d" % i) for i in range(8)]
        ptr = [k.ps("ptr%d" % i, [128, 1024], BF16) for i in range(2)]
        ropeg = k.sb("ropeg", [128, 2, 1024], F32)
        wst = [k.sb("wst%d" % i, [128, 16, 256], F32) for i in range(2)]
        wbf = [k.sb("wbf%d" % i, [128, 16, 256], BF16) for i in range(2)]
        pfm = [k.ps("pfm%d" % i, [128, 512]) for i in range(2)]
        psw = [k.ps("psw%d" % i, [128, 512]) for i in range(2)]
        ptm = [k.ps("ptm%d" % i, [128, 256]) for i in range(2)]
        qraw = [k.sb("qraw%d" % i, [128, 512], F32) for i in range(2)]
        t1 = [k.sb("t1_%d" % i, [128, 512], F32) for i in range(2)]
        t2 = [k.sb("t2_%d" % i, [128, 512], F32) for i in range(2)]
        stg = [k.sb("stg%d" % i, [128, 1024], BF16) for i in range(2)]
        stm = [k.sb("stm%d" % i, [128, 8, 256], BF16) for i in range(2)]
        stmf = k.sb("stmf", [128, 8, 36], F32)
        cnt = {"seg": 0, "fm": 0, "tm": 0, "st": 0, "sm": 0}

        kv_segs = []
        for i in range(3):
            kv_segs.append((768 + 256 * i, 256, "fm", True, "kTA", 256 * i))
        for i in range(3):
            kv_segs.append((1536 + 256 * i, 256, "tm", False, "vA", 256 * i))
        kv_segs.append((3072, 256, "fm", False, "cT", 0))
        kv_segs.append((3328, 128, "fm", False, "cT", 256))
        kv_segs.append((3456, 192, "fm", True, "kTS", 0))
        kv_segs.append((3648, 192, "tm", False, "vS", 0))
        kv_segs.append((3840, 192, "fm", True, "kTW", 0))
        kv_segs.append((4032, 192, "tm", False, "vW", 0))
        for i in range(2):
            kv_segs.append((4772 + 256 * i, 256, "fm", False, "kTC", 256 * i))
        for i in range(2):
            kv_segs.append((5284 + 256 * i, 256, "tm", False, "vC", 256 * i))
        q_segs = []
        for i in range(3):
            q_segs.append((256 * i, 256, "fm", True, "qTA", 256 * i))
        for i in range(3):
            q_segs.append((2304 + 256 * i, 256, "fm", True, "qTB", 256 * i))
        q_segs.append((4224, 36, "gate", False, "gates", 0))
        for i in range(2):
            q_segs.append((4260 + 256 * i, 256, "fm", False, "qTC", 256 * i))

        for g in range(5):
            own = (g == 4)
            k.dma("act", [(ropeg.t[:, 0, :], I["rope"][0, :, g * 1024:(g + 1) * 1024]),
                          (ropeg.t[:, 1, :], I["rope"][1, :, g * 1024:(g + 1) * 1024])], writes=[ropeg])
            for tt in range(8):
                x = xt[tt % 2]
                r0 = g * 1024 + tt * 128
                k.dma("sp", [(x.t[:], I["xin"][r0:r0 + 128, :])], writes=[x])
                h = hb[tt % 2]
                self.norm_tile(x, gamb, shb, h, ms[tt % 2], junk, tmp)
                for half in range(2):
                    p = ptr[half]
                    for kk in range(8):
                        kc = half * 8 + kk
                        k.op("pe", lambda e, p=p, kk=kk, kc=kc, h=h: e.transpose(
                            p.t[:, kk * 128:(kk + 1) * 128], h.t[:, kc * 128:(kc + 1) * 128], identb.t[:]),
                            reads=[h, identb], writes=[p])
                    dst = hT.t[:, half * 8:(half + 1) * 8, tt * 128:(tt + 1) * 128]
                    src = p.t[:].rearrange("p (a b) -> p a b", a=8)
                    eng = "act" if half == 0 else "dve"
                    if eng == "act":
                        k.op("act", lambda e, dst=dst, src=src: e.copy(dst, src), reads=[p], writes=[hTb[tt]])
                    else:
                        k.op("dve", lambda e, dst=dst, src=src: e.tensor_copy(dst, src), reads=[p], writes=[hTb[tt]])
            for (c0, w, kind, rope, dname, doff) in (q_segs if own else kv_segs):
                si = cnt["seg"] % 2
                cnt["seg"] += 1
                ws, wb = wst[si], wbf[si]
                src = I["w_in"][:, c0:c0 + w].rearrange("(k p) n -> p k n", p=128)
                k.dma("sp", [(ws.t[:, 4 * i:4 * i + 4, 0:w], src[:, 4 * i:4 * i + 4, :]) for i in range(4)], writes=[ws])
                k.op("pool", lambda e, ws=ws, wb=wb, w=w: e.tensor_copy(wb.t[:, :, 0:w], ws.t[:, :, 0:w]),
                     reads=[ws], writes=[wb])
                dst = S[dname]
                if kind == "fm":
                    u0 = 0
                    while u0 < w:
                        uw = min(128, w - u0)
                        sg = stg[cnt["st"] % 2]
                        cnt["st"] += 1
                        for half in range(2):
                            p = pfm[cnt["fm"] % 2]
                            fi = cnt["fm"] % 2
                            cnt["fm"] += 1
                            for kk in range(16):
                                k.op("pe", lambda e, p=p, kk=kk, wb=wb, u0=u0, uw=uw, half=half: e.matmul(
                                    p.t[0:uw, :], wb.t[:, kk, u0:u0 + uw], hT.t[:, kk, half * 512:(half + 1) * 512],
                                    start=(kk == 0), stop=(kk == 15)),
                                    reads=[wb] + hTb[4 * half:4 * half + 4], writes=[p])
                            so = sg.t[0:uw, half * 512:(half + 1) * 512]
                            if not rope:
                                k.op("act", lambda e, so=so, p=p, uw=uw: e.copy(so, p.t[0:uw, :]), reads=[p], writes=[sg])
                            else:
                                qr, pw_, a1, a2 = qraw[fi], psw[fi], t1[fi], t2[fi]
                                k.op("act", lambda e, qr=qr, p=p, uw=uw: e.copy(qr.t[0:uw, :], p.t[0:uw, :]),
                                     reads=[p], writes=[qr])
                                k.op("pe", lambda e, pw_=pw_, qr=qr, uw=uw: e.matmul(
                                    pw_.t[0:uw, :], pswap.t[0:uw, 0:uw], qr.t[0:uw, :], start=True, stop=True),
                                    reads=[pswap, qr], writes=[pw_])
                                cs = ropeg.t[0:uw, 0, half * 512:(half + 1) * 512]
                                sn = ropeg.t[0:uw, 1, half * 512:(half + 1) * 512]
                                k.op("pool", lambda e, a1=a1, qr=qr, cs=cs, uw=uw: e.tensor_tensor(
                                    a1.t[0:uw, :], qr.t[0:uw, :], cs, ALU.mult), reads=[qr, ropeg], writes=[a1])
                                k.op("dve", lambda e, a2=a2, pw_=pw_, sn=sn, uw=uw: e.tensor_tensor(
                                    a2.t[0:uw, :], pw_.t[0:uw, :], sn, ALU.mult), reads=[pw_, ropeg], writes=[a2])
                                k.op("pool", lambda e, so=so, a1=a1, a2=a2, uw=uw: e.tensor_tensor(
                                    so, a1.t[0:uw, :], a2.t[0:uw, :], ALU.add), reads=[a1, a2], writes=[sg])
                        tok0 = 0 if own else g * 1024
                        k.dma("act", [(dst[doff + u0:doff + u0 + uw, tok0:tok0 + 1024], sg.t[0:uw, :])],
                              reads=[sg], writes=[self.SB[dname]])
                        u0 += uw
                else:
                    sm = stm[cnt["sm"] % 2] if kind == "tm" else stmf
                    cnt["sm"] += 1
                    for tt in range(8):
                        p = ptm[cnt["tm"] % 2]
                        cnt["tm"] += 1
                        for kk in range(16):
                            k.op("pe", lambda e, p=p, kk=kk, wb=wb, w=w, tt=tt: e.matmul(
                                p.t[:, 0:w], hT.t[:, kk, tt * 128:(tt + 1) * 128], wb.t[:, kk, 0:w],
                                start=(kk == 0), stop=(kk == 15)), reads=[wb, hTb[tt]], writes=[p])
                        if kind == "tm":
                            gt = g * 8 + tt
                            k.op("dve", lambda e, sm=sm, p=p, w=w, tt=tt, gt=gt: e.tensor_scalar(
                                sm.t[:, tt, 0:w], p.t[:, 0:w], kval.t[:, gt:gt + 1], None, op0=ALU.mult),
                                reads=[p, kval], writes=[sm])
                        else:
                            k.op("act", lambda e, sm=sm, p=p, w=w, tt=tt: e.activation(
                                sm.t[:, tt, 0:w], p.t[:, 0:w], AF.Sigmoid), reads=[p], writes=[sm])
                    tok0 = 0 if own else g * 1024
                    dv = dst[tok0:tok0 + 1024, doff:doff + w].rearrange("(t p) w -> p t w", p=128)
                    k.dma("act", [(dv, sm.t[:, :, 0:w])], reads=[sm], writes=[self.SB[dname]])

    def load_head(self, slot, kname, vname, qname, h, vcols=65, need_ones=True):
        k, S = self.k, self.S
        kT, va, qT = slot
        k.dma("sp", [(kT.t[:, i * 1024:(i + 1) * 1024], S[kname][h * 64:(h + 1) * 64, i * 1024:(i + 1) * 1024])
                     for i in range(4)], reads=[self.SB[kname]], writes=[kT])
        vsrc = S[vname][:, h * 64:(h + 1) * 64].rearrange("(t p) c -> p t c", p=128)
        k.dma("sp", [(va.t[:, i * 8:(i + 1) * 8, 0:64], vsrc[:, i * 8:(i + 1) * 8, :]) for i in range(4)],
              reads=[self.SB[vname]], writes=[va])
        if need_ones:
            k.op("pool", lambda e: e.tensor_copy(va.t[:, :, 64], self.kval.t[:, :]), reads=[self.kval, va], writes=[va])
        k.dma("sp", [(qT.t[:], S[qname][h * 64:(h + 1) * 64, :])], reads=[self.SB[qname]], writes=[qT])

    def head_slots(self, n=2):
        k = self.k
        return [(k.sb("kT%d" % i, [64, TL], BF16), k.sb("va%d" % i, [128, NT, 65], BF16),
                 k.sb("qT%d" % i, [64, NQ], BF16)) for i in range(n)]

    def norm_store(self, ost, nh, hg0):
        k, S = self.k, self.S
        W = nh * 64
        mixb = k.sb("mixb", [128, W], F32)
        k.dma("sp", [(mixb.t[:], self.I["mix_norm"][hg0 * 64:hg0 * 64 + W].partition_broadcast(128))], writes=[mixb])
        sq = k.sb("nsq", [128, W], F32)
        ss = k.sb("nss", [128, 3, nh], F32)
        on = [k.sb("non%d" % i, [128, W], BF16) for i in range(2)]
        for qt in range(8):
            o = ost.t[:, qt, :]
            k.op("dve", lambda e, o=o: e.tensor_tensor(sq.t[:], o, o, ALU.mult), reads=[ost], writes=[sq])
            k.op("dve", lambda e: e.tensor_reduce(ss.t[:, 0, :], sq.t[:].rearrange("p (h d) -> p h d", d=64),
                                                  AX.X, ALU.add), reads=[sq], writes=[ss])
            k.op("act", lambda e: e.activation(ss.t[:, 1, :], ss.t[:, 0, :], AF.Sqrt, bias=EPS, scale=1.0 / 64),
                 reads=[ss], writes=[ss])
            k.op("dve", lambda e: e.reciprocal(ss.t[:, 2, :], ss.t[:, 1, :]), reads=[ss], writes=[ss])
            rb = ss.t[:, 2, :].unsqueeze(2).to_broadcast([128, nh, 64])
            k.op("dve", lambda e, o=o, rb=rb: e.tensor_tensor(sq.t[:].rearrange("p (h d) -> p h d", d=64),
                                                             o.rearrange("p (h d) -> p h d", d=64), rb, ALU.mult),
                 reads=[ost, ss], writes=[sq])
            ob = on[qt % 2]
            k.op("pool", lambda e, ob=ob: e.tensor_tensor(ob.t[:], sq.t[:], mixb.t[:], ALU.mult),
                 reads=[sq, mixb], writes=[ob])
            k.dma("act", [(S["otok"][qt * 128:(qt + 1) * 128, hg0 * 64:hg0 * 64 + W], ob.t[:])], reads=[ob],
                  writes=[self.SB["otok"]])

    def softmax_branch(self, kT, va, qT, qg, jlist, maskfn, acc, st, E, Em, cnt, vw=65, qap=None):
        k = self.k
        if qap is None:
            qap = qT.t[:, qg * 512:(qg + 1) * 512]
        for ji, j in enumerate(jlist):
            s_ = st[cnt[0] % 2]
            e_ = E[cnt[0] % 2]
            m_ = Em[cnt[0] % 2]
            cnt[0] += 1
            k.op("pe", lambda e, s_=s_, j=j: e.matmul(s_.t[:], kT.t[:, j * 128:(j + 1) * 128],
                                                      qap, start=True, stop=True),
                 reads=[kT, qT], writes=[s_])
            k.op("act", lambda e, s_=s_, e_=e_: e.activation(e_.t[:], s_.t[:], AF.Exp, scale=SCALE),
                 reads=[s_], writes=[e_])
            mk = maskfn(j)
            if mk is not None:
                mt, map_ = mk
                k.op("dve", lambda e, m_=m_, e_=e_, map_=map_: e.tensor_tensor(m_.t[:], e_.t[:], map_, ALU.mult),
                     reads=[e_, mt], writes=[m_])
                src = m_
            else:
                src = e_
            for qt in range(4):
                k.op("pe", lambda e, src=src, qt=qt, j=j, ji=ji: e.matmul(
                    acc.t[:, qt, 0:vw], src.t[:, qt * 128:(qt + 1) * 128], va.t[:, j, 0:vw],
                    start=(ji == 0 and qt == 0), stop=(ji == len(jlist) - 1), skip_group_check=True),
                     reads=[src, va], writes=[acc])

    def phase_att_a(self):
        k = self.k
        self.kval = self.load_const("kvalid")
        maskA = self.load_const("maskA", rearr="a p n -> p a n")
        slots = self.head_slots()
        st = [k.ps("st%d" % i, [128, 512]) for i in range(2)]
        acc = [k.ps("acc%d" % i, [128, 4, 128]) for i in range(2)]
        E = [k.sb("E%d" % i, [128, 512], BF16) for i in range(2)]
        Em = [k.sb("Em%d" % i, [128, 512], BF16) for i in range(2)]
        ost = k.sb("ostA", [128, 8, 768], F32)
        rec = k.sb("rec", [128, 4], F32)
        cnt = [0]
        self.load_head(slots[0], "kTA", "vA", "qTA", 0)
        for h in range(12):
            if h + 1 < 12:
                self.load_head(slots[(h + 1) % 2], "kTA", "vA", "qTA", h + 1)
            kT, va, qT = slots[h % 2]
            for qg in range(2):
                i0 = 24 + 4 * qg
                a = acc[(2 * h + qg) % 2]
                self.softmax_branch(kT, va, qT, qg, list(range(i0 - 16, i0 + 4)),
                                    lambda j: (maskA, maskA.t[:, i0 - j + 3, :]), a, st, E, Em, cnt)
                k.op("dve", lambda e, a=a: e.reciprocal(rec.t[:], a.t[:, :, 64]), reads=[a], writes=[rec])
                for qt in range(4):
                    k.op("dve", lambda e, a=a, qt=qt, h=h, qg=qg: e.tensor_scalar(
                        ost.t[:, qg * 4 + qt, h * 64:(h + 1) * 64], a.t[:, qt, 0:64], rec.t[:, qt:qt + 1], None,
                        op0=ALU.mult), reads=[a, rec], writes=[ost])
        self.norm_store(ost, 12, 0)

    def phase_att_c(self):
        k = self.k
        self.kval = self.load_const("kvalid")
        u2 = self.load_const("u2")
        onesf = self.load_const("onesf")
        mSF = self.load_const("maskStrictF", rearr="a p n -> p a n")
        mSB = self.load_const("maskStrictB", rearr="a p n -> p a n")
        slots = self.head_slots()
        zp = [k.ps("zp%d" % i, [128, 512]) for i in range(2)]
        sp = [k.ps("sp%d" % i, [128, 512]) for i in range(2)]
        acc = [k.ps("accc%d" % i, [128, 4, 128]) for i in range(2)]
        e1 = [k.sb("e1_%d" % i, [128, 512], F32) for i in range(2)]
        Lp = [k.sb("Lp%d" % i, [128, 512], F32) for i in range(2)]
        Lm = [k.sb("Lm%d" % i, [128, 512], F32) for i in range(2)]
        t1 = [k.sb("ct1_%d" % i, [128, 512], F32) for i in range(2)]
        arg = [k.sb("arg%d" % i, [128, 512], F32) for i in range(2)]
        av = [k.sb("av%d" % i, [128, 512], BF16) for i in range(2)]
        am = [k.sb("am%d" % i, [128, 512], BF16) for i in range(2)]
        Lacc = k.sb("Lacc", [128, 512], F32)
        ost = k.sb("ostC", [128, 8, 512], F32)
        c = 0
        self.load_head(slots[0], "kTC", "vC", "qTC", 0, need_ones=False)
        for h in range(8):
            if h + 1 < 8:
                self.load_head(slots[(h + 1) % 2], "kTC", "vC", "qTC", h + 1, need_ones=False)
            kT, va, qT = slots[h % 2]
            for qg in range(2):
                i0 = 24 + 4 * qg
                a_ = acc[(2 * h + qg) % 2]
                jl = list(range(i0 + 3, -1, -1))
                k.op("pool", lambda e: e.memset(Lacc.t[:], 0.0), writes=[Lacc])
                for ji, j in enumerate(jl):
                    b = c % 2
                    c += 1
                    z, s_ = zp[b], sp[b]
                    k.op("pe", lambda e, z=z, j=j: e.matmul(z.t[:], kT.t[:, j * 128:(j + 1) * 128],
                                                            qT.t[:, qg * 512:(qg + 1) * 512], start=True, stop=True),
                         reads=[kT, qT], writes=[z])
                    k.op("act", lambda e, z=z, b=b: e.activation(e1[b].t[:], z.t[:], AF.Exp, scale=SCALE),
                         reads=[z], writes=[e1[b]])
                    k.op("act", lambda e, b=b: e.activation(Lp[b].t[:], e1[b].t[:], AF.Ln, bias=1.0, scale=1.0),
                         reads=[e1[b]], writes=[Lp[b]])
                    if j >= i0:
                        k.op("dve", lambda e, b=b, j=j: e.tensor_tensor(Lm[b].t[:], Lp[b].t[:], mSF.t[:, j - i0, :], ALU.mult),
                             reads=[Lp[b], mSF], writes=[Lm[b]])
                    else:
                        k.op("dve", lambda e, b=b, j=j: e.tensor_scalar(Lm[b].t[:], Lp[b].t[:], self.kval.t[:, j:j + 1], None,
                                                                        op0=ALU.mult), reads=[Lp[b], self.kval], writes=[Lm[b]])
                    k.op("pe", lambda e, s_=s_, b=b, ji=ji: e.matmul(s_.t[:], u2.t[:], Lm[b].t[:], start=True, stop=(ji == 0)),
                         reads=[u2, Lm[b]], writes=[s_])
                    if ji > 0:
                        k.op("pe", lambda e, s_=s_: e.matmul(s_.t[:], onesf.t[:], Lacc.t[:], start=False, stop=True),
                             reads=[onesf, Lacc], writes=[s_])
                    k.op("dve", lambda e, z=z, b=b: e.scalar_tensor_tensor(t1[b].t[:], z.t[:], SCALE, Lp[b].t[:],
                                                                           ALU.mult, ALU.subtract),
                         reads=[z, Lp[b]], writes=[t1[b]])
                    k.op("dve", lambda e, s_=s_, b=b: e.tensor_tensor(arg[b].t[:], t1[b].t[:], s_.t[:], ALU.subtract),
                         reads=[t1[b], s_], writes=[arg[b]])
                    k.op("act", lambda e, b=b: e.activation(av[b].t[:], arg[b].t[:], AF.Exp), reads=[arg[b]], writes=[av[b]])
                    src = av[b]
                    if j >= i0:
                        k.op("pool", lambda e, b=b, j=j: e.tensor_tensor(am[b].t[:], av[b].t[:], mSB.t[:, j - i0, :], ALU.mult),
                             reads=[av[b], mSB], writes=[am[b]])
                        src = am[b]
                    k.op("pool", lambda e, b=b: e.tensor_tensor(Lacc.t[:], Lacc.t[:], Lm[b].t[:], ALU.add),
                         reads=[Lacc, Lm[b]], writes=[Lacc])
                    for qt in range(4):
                        k.op("pe", lambda e, src=src, qt=qt, j=j, ji=ji: e.matmul(
                            a_.t[:, qt, 0:64], src.t[:, qt * 128:(qt + 1) * 128], va.t[:, j, 0:64],
                            start=(ji == 0 and qt == 0), stop=(ji == len(jl) - 1), skip_group_check=True),
                            reads=[src, va], writes=[a_])
                k.op("act", lambda e, a_=a_, h=h, qg=qg: e.copy(ost.t[:, qg * 4:(qg + 1) * 4, h * 64:(h + 1) * 64],
                                                             a_.t[:, :, 0:64]), reads=[a_], writes=[ost])
        self.norm_store(ost, 8, 24)

    def phase_att_b(self):
        k, I, S = self.k, self.I, self.S
        self.kval = self.load_const("kvalid")
        cval = self.load_const("cvalid")
        ovl = self.load_const("ovl")
        identb = self.load_const("identb")
        mCmp = self.load_const("maskCmp", rearr="a b p n -> p (a b) n")
        mCaus = self.load_const("maskCaus", rearr="a p n -> p a n")
        mW = self.load_const("maskW", rearr="a p n -> p a n")
        expand = self.load_const("expand")
        impb_t = k.sb("impbias", [128, 8, 64], F32)
        k.dma("sp", [(impb_t.t[:], I["impbias"].rearrange("(t p) m -> p t m", p=128))], writes=[impb_t])
        gates = k.sb("gates", [128, 8, 36], F32)
        k.dma("sp", [(gates.t[:], S["gates"].rearrange("(t p) c -> p t c", p=128))], reads=[self.SB["gates"]],
              writes=[gates])
        kcT = k.sb("kcT", [64, 3, 256], BF16)
        rhsC = k.sb("rhsC", [128, 3, 2, 129], BF16)
        k.op("pool", lambda e: e.memset(kcT.t[:], 0.0), writes=[kcT])
        for g in range(3):
            k.op("pool", lambda e, g=g: e.tensor_copy(rhsC.t[:, g, :, 64], cval.t[:, :]), reads=[cval], writes=[rhsC])
            k.op("pool", lambda e, g=g: e.tensor_copy(rhsC.t[:, g, :, 65:129], ovl.t[:, :, :]), reads=[ovl], writes=[rhsC])
        with k.scope():
            pswap = self.load_const("pswap")
            ropec = k.sb("ropec", [64, 2, 256], F32)
            k.dma("sp", [(ropec.t[:, 0, :], I["ropec"][0]), (ropec.t[:, 1, :], I["ropec"][1])], writes=[ropec])
            w1f = k.sb("w1f", [64, 32, 128], F32)
            w1b = k.sb("w1b", [64, 32, 128], BF16)
            w2f = k.sb("w2f", [128, 64], F32)
            w2b = k.sb("w2b", [128, 64], BF16)
            pef = k.sb("pef", [64, 32], F32)
            peb = k.sb("peb", [64, 32], BF16)
            cb = k.sb("cb", [128, 1], F32)
            xk = [k.sb("xk%d" % i, [64, TL], BF16) for i in range(2)]
            u = k.sb("cu", [128, 255], F32)
            uu = k.sb("cuu", [128, 255], F32)
            sg = k.sb("csg", [128, 255], F32)
            hid = k.sb("hid", [128, 256], BF16)
            kraw = k.sb("kraw", [64, 255], F32)
            ka1 = k.sb("ka1", [64, 255], F32)
            ka2 = k.sb("ka2", [64, 255], F32)
            pb = k.ps("pcb", [128, 1])
            hp = k.ps("hp", [128, 255])
            kp = k.ps("kp", [64, 255])
            kw = k.ps("kw", [64, 255])
            vp = [k.ps("vp%d" % i, [128, 64]) for i in range(2)]
            k.op("pool", lambda e: e.memset(hid.t[:], 0.0), writes=[hid])
            ci = 0
            for kv in range(2):
                k.dma("sp", [(w1f.t[:, 8 * i:8 * i + 8, :],
                              I["cmp_w1"][kv].rearrange("(l d) h -> d l h", d=64)[:, 8 * i:8 * i + 8, :]) for i in range(4)],
                      writes=[w1f])
                k.op("pool", lambda e: e.tensor_copy(w1b.t[:], w1f.t[:]), reads=[w1f], writes=[w1b])
                k.dma("sp", [(w2f.t[:], I["cmp_w2"][kv])], writes=[w2f])
                k.op("pool", lambda e: e.tensor_copy(w2b.t[:], w2f.t[:]), reads=[w2f], writes=[w2b])
                k.dma("sp", [(pef.t[:], I["cmp_pe"][kv].rearrange("l d -> d l"))], writes=[pef],
                      allow_slow_non_contiguous=True)
                k.op("pool", lambda e: e.tensor_copy(peb.t[:], pef.t[:]), reads=[pef], writes=[peb])
                for l in range(32):
                    k.op("pe", lambda e, l=l: e.matmul(pb.t[:], w1b.t[:, l, :], peb.t[:, l:l + 1],
                                                       start=(l == 0), stop=(l == 31)), reads=[w1b, peb], writes=[pb])
                k.op("act", lambda e: e.copy(cb.t[:], pb.t[:]), reads=[pb], writes=[cb])
                for g in range(3):
                    x_ = xk[ci % 2]
                    ci += 1
                    r0 = kv * 192 + g * 64
                    k.dma("sp", [(x_.t[:, i * 1024:(i + 1) * 1024], S["cT"][r0:r0 + 64, i * 1024:(i + 1) * 1024])
                                 for i in range(4)], reads=[self.SB["cT"]], writes=[x_])
                    for l in range(32):
                        k.op("pe", lambda e, l=l, x_=x_: e.matmul(hp.t[:], w1b.t[:, l, :], x_.t[:, l:l + 4065:16],
                                                                  start=(l == 0), stop=(l == 31)),
                             reads=[w1b, x_], writes=[hp])
                    k.op("act", lambda e: e.activation(u.t[:], hp.t[:], AF.Identity, bias=cb.t[:, 0:1], scale=1.0),
                         reads=[hp, cb], writes=[u])
                    k.op("dve", lambda e: e.tensor_tensor(uu.t[:], u.t[:], u.t[:], ALU.mult), reads=[u], writes=[uu])
                    k.op("dve", lambda e: e.tensor_scalar(uu.t[:], uu.t[:], 0.044715, 1.0, op0=ALU.mult, op1=ALU.add),
                         reads=[uu], writes=[uu])
                    k.op("dve", lambda e: e.tensor_tensor(uu.t[:], uu.t[:], u.t[:], ALU.mult), reads=[uu, u], writes=[uu])
                    k.op("act", lambda e: e.activation(sg.t[:], uu.t[:], AF.Sigmoid, scale=1.5957691216057308),
                         reads=[uu], writes=[sg])
                    k.op("dve", lambda e: e.tensor_tensor(hid.t[:, 0:255], u.t[:], sg.t[:], ALU.mult),
                         reads=[u, sg], writes=[hid])
                    if kv == 0:
                        k.op("pe", lambda e: e.matmul(kp.t[:], w2b.t[:], hid.t[:, 0:255], start=True, stop=True),
                             reads=[w2b, hid], writes=[kp])
                        k.op("act", lambda e: e.copy(kraw.t[:], kp.t[:]), reads=[kp], writes=[kraw])
                        k.op("pe", lambda e: e.matmul(kw.t[:], pswap.t[0:64, 0:64], kraw.t[:], start=True, stop=True),
                             reads=[pswap, kraw], writes=[kw])
                        k.op("pool", lambda e: e.tensor_tensor(ka1.t[:], kraw.t[:], ropec.t[:, 0, 0:255], ALU.mult),
                             reads=[kraw, ropec], writes=[ka1])
                        k.op("dve", lambda e: e.tensor_tensor(ka2.t[:], kw.t[:], ropec.t[:, 1, 0:255], ALU.mult),
                             reads=[kw, ropec], writes=[ka2])
                        k.op("pool", lambda e, g=g: e.tensor_tensor(kcT.t[:, g, 0:255], ka1.t[:], ka2.t[:], ALU.add),
                             reads=[ka1, ka2], writes=[kcT])
                    else:
                        for nt in range(2):
                            v_ = vp[nt]
                            k.op("pe", lambda e, nt=nt, v_=v_: e.matmul(v_.t[:], hid.t[:, nt * 128:(nt + 1) * 128], w2b.t[:],
                                                                        start=True, stop=True), reads=[hid, w2b], writes=[v_])
                            k.op("dve", lambda e, nt=nt, g=g, v_=v_: e.tensor_scalar(
                                rhsC.t[:, g, nt, 0:64], v_.t[:], cval.t[:, nt:nt + 1], None, op0=ALU.mult),
                                reads=[v_, cval], writes=[rhsC])
        kS = k.sb("kS", [64, TL], BF16)
        kW = k.sb("kW", [64, TL], BF16)
        vaS = k.sb("vaS", [128, NT, 65], BF16)
        vaW = k.sb("vaW", [128, NT, 65], BF16)
        qT4 = k.sb("qT4", [64, 4, 512], BF16)
        st = [k.ps("st%d" % i, [128, 512]) for i in range(2)]
        accC = k.ps("accC", [128, 4, 256])
        accS = k.ps("accS", [128, 4, 128])
        accW = k.ps("accW", [128, 4, 128])
        mp = k.ps("mp", [128, 512])
        selTp = k.ps("selTp", [64, 512], BF16)
        E = [k.sb("E%d" % i, [128, 512], BF16) for i in range(2)]
        Em = [k.sb("Em%d" % i, [128, 512], BF16) for i in range(2)]
        maskall = k.sb("maskall", [128, NT, 512], BF16)
        maskb = [Buf("maskall%d" % j) for j in range(NT)]
        ocmp = k.sb("ocmp", [128, 4, 4, 64], F32)
        impg = k.sb("impg", [128, 4, 64], F32)
        recC = k.sb("recC", [128, 4], F32)
        recS = k.sb("recS", [128, 2, 4], F32)
        wsw = k.sb("wsw", [128, 2, 4], F32)
        impb = k.sb("impb", [128, 64], F32)
        imt = k.sb("imt", [128, 64], F32)
        m8 = k.sb("m8", [128, 2, 8], F32)
        selb = k.sb("selb", [128, 64], BF16)
        selT = k.sb("selT", [64, 512], BF16)
        otmp = k.sb("otmp", [128, 64], F32)
        ost = k.sb("ostB", [128, 8, 768], F32)
        cnt = [0]
        for g in range(3):
            r0 = g * 64
            k.dma("sp", [(kS.t[:, i * 1024:(i + 1) * 1024], S["kTS"][r0:r0 + 64, i * 1024:(i + 1) * 1024]) for i in range(4)],
                  reads=[self.SB["kTS"]], writes=[kS])
            k.dma("sp", [(kW.t[:, i * 1024:(i + 1) * 1024], S["kTW"][r0:r0 + 64, i * 1024:(i + 1) * 1024]) for i in range(4)],
                  reads=[self.SB["kTW"]], writes=[kW])
            for (va, nm) in ((vaS, "vS"), (vaW, "vW")):
                vsrc = S[nm][:, r0:r0 + 64].rearrange("(t p) c -> p t c", p=128)
                k.dma("sp", [(va.t[:, i * 8:(i + 1) * 8, 0:64], vsrc[:, i * 8:(i + 1) * 8, :]) for i in range(4)],
                      reads=[self.SB[nm]], writes=[va])
                k.op("pool", lambda e, va=va: e.tensor_copy(va.t[:, :, 64], self.kval.t[:, :]), reads=[self.kval, va],
                     writes=[va])
            for qg in range(2):
                i0 = 24 + 4 * qg
                k.dma("sp", [(qT4.t[:, r, :], S["qTB"][(4 * g + r) * 64:(4 * g + r + 1) * 64, qg * 512:(qg + 1) * 512])
                             for r in range(4)], reads=[self.SB["qTB"]], writes=[qT4])
                for r in range(4):
                    for nt in range(2):
                        s_ = st[cnt[0] % 2]
                        e_ = E[cnt[0] % 2]
                        m_ = Em[cnt[0] % 2]
                        cnt[0] += 1
                        k.op("pe", lambda e, s_=s_, nt=nt, r=r: e.matmul(s_.t[:], kcT.t[:, g, nt * 128:(nt + 1) * 128],
                                                                         qT4.t[:, r, :], start=True, stop=True),
                             reads=[kcT, qT4], writes=[s_])
                        k.op("act", lambda e, s_=s_, e_=e_: e.activation(e_.t[:], s_.t[:], AF.Exp, scale=SCALE),
                             reads=[s_], writes=[e_])
                        k.op("dve", lambda e, m_=m_, e_=e_, nt=nt: e.tensor_tensor(m_.t[:], e_.t[:], mCmp.t[:, qg * 2 + nt, :],
                                                                                  ALU.mult), reads=[e_, mCmp], writes=[m_])
                        for qt in range(4):
                            k.op("pe", lambda e, m_=m_, qt=qt, nt=nt: e.matmul(
                                accC.t[:, qt, 0:129], m_.t[:, qt * 128:(qt + 1) * 128], rhsC.t[:, g, nt, :],
                                start=(nt == 0 and qt in (0, 2)), stop=(nt == 1), skip_group_check=True),
                                reads=[m_, rhsC], writes=[accC])
                    k.op("dve", lambda e: e.tensor_scalar(recC.t[:], accC.t[:, :, 64], 1e-30, None, op0=ALU.max),
                         reads=[accC], writes=[recC])
                    k.op("dve", lambda e: e.reciprocal(recC.t[:], recC.t[:]), reads=[recC], writes=[recC])
                    for qt in range(4):
                        k.op("dve", lambda e, r=r, qt=qt: e.tensor_scalar(ocmp.t[:, r, qt, :], accC.t[:, qt, 0:64],
                                                                          recC.t[:, qt:qt + 1], None, op0=ALU.mult),
                             reads=[accC, recC], writes=[ocmp])
                        if r == 0:
                            k.op("dve", lambda e, qt=qt: e.tensor_scalar(impg.t[:, qt, :], accC.t[:, qt, 65:129],
                                                                         recC.t[:, qt:qt + 1], None, op0=ALU.mult),
                                 reads=[accC, recC], writes=[impg])
                        else:
                            k.op("dve", lambda e, qt=qt: e.scalar_tensor_tensor(
                                impg.t[:, qt, :], accC.t[:, qt, 65:129], recC.t[:, qt:qt + 1], impg.t[:, qt, :],
                                ALU.mult, ALU.add), reads=[accC, recC, impg], writes=[impg])
                for qt in range(4):
                    k.op("dve", lambda e, qt=qt: e.tensor_tensor(impb.t[:], impg.t[:, qt, :], impb_t.t[:, qg * 4 + qt, :],
                                                                 ALU.add), reads=[impg, impb_t], writes=[impb])
                    k.op("dve", lambda e: e.max(m8.t[:, 0, :], impb.t[:]), reads=[impb], writes=[m8])
                    k.op("dve", lambda e: e.match_replace(imt.t[:], m8.t[:, 0, :], impb.t[:], -1e30),
                         reads=[m8, impb], writes=[imt])
                    k.op("dve", lambda e: e.max(m8.t[:, 1, :], imt.t[:]), reads=[imt], writes=[m8])
                    k.op("dve", lambda e: e.tensor_scalar(selb.t[:], impb.t[:], m8.t[:, 1, 7:8], None, op0=ALU.is_ge),
                         reads=[impb, m8], writes=[selb])
                    k.op("pe", lambda e, qt=qt: e.transpose(selTp.t[:, qt * 128:(qt + 1) * 128], selb.t[:], identb.t[:]),
                         reads=[selb, identb], writes=[selTp])
                k.op("act", lambda e: e.copy(selT.t[:], selTp.t[:]), reads=[selTp], writes=[selT])
                for j in range(i0 + 4):
                    k.op("pe", lambda e, j=j: e.matmul(mp.t[:], expand.t[:, j * 128:(j + 1) * 128], selT.t[:],
                                                       start=True, stop=True), reads=[expand, selT], writes=[mp])
                    if j >= i0:
                        k.op("dve", lambda e, j=j: e.tensor_tensor(maskall.t[:, j, :], mp.t[:], mCaus.t[:, j - i0, :], ALU.mult),
                             reads=[mp, mCaus], writes=[maskb[j]])
                    else:
                        k.op("act", lambda e, j=j: e.copy(maskall.t[:, j, :], mp.t[:]), reads=[mp], writes=[maskb[j]])
                for r in range(4):
                    h = 4 * g + r
                    self.softmax_branch(kS, vaS, qT4, qg, list(range(i0 + 4)),
                                        lambda j: (maskb[j], maskall.t[:, j, :]), accS, st, E, Em, cnt, qap=qT4.t[:, r, :])
                    self.softmax_branch(kW, vaW, qT4, qg, list(range(i0 - 4, i0 + 4)),
                                        lambda j: (mW, mW.t[:, j - i0 + 4, :]), accW, st, E, Em, cnt, qap=qT4.t[:, r, :])
                    k.op("dve", lambda e: e.tensor_scalar(recS.t[:, 0, :], accS.t[:, :, 64], 1e-30, None, op0=ALU.max),
                         reads=[accS], writes=[recS])
                    k.op("dve", lambda e: e.tensor_scalar(recS.t[:, 1, :], accW.t[:, :, 64], 1e-30, None, op0=ALU.max),
                         reads=[accW], writes=[recS])
                    k.op("dve", lambda e: e.reciprocal(recS.t[:], recS.t[:]), reads=[recS], writes=[recS])
                    for br in (1, 2):
                        k.op("dve", lambda e, br=br, h=h: e.tensor_tensor(
                            wsw.t[:, br - 1, :], recS.t[:, br - 1, :], gates.t[:, qg * 4:(qg + 1) * 4, h * 3 + br], ALU.mult),
                            reads=[recS, gates], writes=[wsw])
                    for qt in range(4):
                        qq = qg * 4 + qt
                        k.op("dve", lambda e, r=r, qt=qt, qq=qq, h=h: e.tensor_scalar(
                            otmp.t[:], ocmp.t[:, r, qt, :], gates.t[:, qq, h * 3:h * 3 + 1], None, op0=ALU.mult),
                            reads=[ocmp, gates], writes=[otmp])
                        k.op("dve", lambda e, qt=qt: e.scalar_tensor_tensor(
                            otmp.t[:], accS.t[:, qt, 0:64], wsw.t[:, 0, qt:qt + 1], otmp.t[:], ALU.mult, ALU.add),
                            reads=[accS, wsw, otmp], writes=[otmp])
                        k.op("dve", lambda e, qt=qt, qq=qq, h=h: e.scalar_tensor_tensor(
                            ost.t[:, qq, h * 64:(h + 1) * 64], accW.t[:, qt, 0:64], wsw.t[:, 1, qt:qt + 1], otmp.t[:],
                            ALU.mult, ALU.add), reads=[accW, wsw, otmp], writes=[ost])
        self.norm_store(ost, 12, 12)

    def phase_oproj(self):
        k, I, S = self.k, self.I, self.S
        identb = self.load_const("identb")
        g1b = self.bvec(2, "g1b")
        woutb = k.sb("woutb", [128, 16, D], BF16)
        wob = [Buf("wob%d" % i) for i in range(8)]
        wst = [k.sb("wost%d" % i, [128, 16, 256], F32) for i in range(2)]
        for c in range(8):
            ws = wst[c % 2]
            src = I["w_out"][:, c * 256:(c + 1) * 256].rearrange("(k p) n -> p k n", p=128)
            k.dma("sp", [(ws.t[:, 4 * i:4 * i + 4, :], src[:, 4 * i:4 * i + 4, :]) for i in range(4)], writes=[ws])
            eng = "pool" if c % 2 == 0 else "act"
            if eng == "pool":
                k.op("pool", lambda e, ws=ws, c=c: e.tensor_copy(woutb.t[:, :, c * 256:(c + 1) * 256], ws.t[:]),
                     reads=[ws], writes=[wob[c]])
            else:
                k.op("act", lambda e, ws=ws, c=c: e.copy(woutb.t[:, :, c * 256:(c + 1) * 256], ws.t[:]),
                     reads=[ws], writes=[wob[c]])
        ot = [k.sb("ot%d" % i, [128, D], BF16) for i in range(2)]
        xq = [k.sb("xq%d" % i, [128, D], F32) for i in range(2)]
        x1 = [k.sb("x1_%d" % i, [128, D], F32) for i in range(2)]
        tmp = k.sb("otmp", [128, 512], F32)
        OT = [k.sb("OT%d" % i, [128, 16, 128], BF16) for i in range(2)]
        ptr = [k.ps("optr%d" % i, [128, 1024], BF16) for i in range(2)]
        po = [k.ps("po%d" % i, [128, 512]) for i in range(2)]
        n = 0
        for qt in range(8):
            o_, x_, x1_, OT_ = ot[qt % 2], xq[qt % 2], x1[qt % 2], OT[qt % 2]
            k.dma("sp", [(o_.t[:], S["otok"][qt * 128:(qt + 1) * 128, :])], reads=[self.SB["otok"]], writes=[o_])
            k.dma("sp", [(x_.t[:], I["xin"][TL + qt * 128:TL + (qt + 1) * 128, :])], writes=[x_])
            for half in range(2):
                p = ptr[half]
                for kk in range(8):
                    kc = half * 8 + kk
                    k.op("pe", lambda e, p=p, kk=kk, kc=kc, o_=o_: e.transpose(
                        p.t[:, kk * 128:(kk + 1) * 128], o_.t[:, kc * 128:(kc + 1) * 128], identb.t[:]),
                        reads=[o_, identb], writes=[p])
                dst = OT_.t[:, half * 8:(half + 1) * 8, :]
                src = p.t[:].rearrange("p (a b) -> p a b", a=8)
                if half == 0:
                    k.op("act", lambda e, dst=dst, src=src: e.copy(dst, src), reads=[p], writes=[OT_])
                else:
                    k.op("dve", lambda e, dst=dst, src=src: e.tensor_copy(dst, src), reads=[p], writes=[OT_])
            for c in range(4):
                p = po[n % 2]
                n += 1
                for kk in range(16):
                    k.op("pe", lambda e, p=p, kk=kk, c=c, OT_=OT_: e.matmul(
                        p.t[:], OT_.t[:, kk, :], woutb.t[:, kk, c * 512:(c + 1) * 512], start=(kk == 0), stop=(kk == 15)),
                        reads=[OT_, wob[2 * c], wob[2 * c + 1]], writes=[p])
                k.op("dve", lambda e, p=p, c=c: e.tensor_tensor(tmp.t[:], p.t[:], g1b.t[:, c * 512:(c + 1) * 512], ALU.mult),
                     reads=[p, g1b], writes=[tmp])
                k.op("pool", lambda e, c=c, x_=x_, x1_=x1_: e.tensor_tensor(
                    x1_.t[:, c * 512:(c + 1) * 512], tmp.t[:], x_.t[:, c * 512:(c + 1) * 512], ALU.add),
                    reads=[tmp, x_], writes=[x1_])
            k.dma("act", [(S["x1"][qt * 128:(qt + 1) * 128, :], x1_.t[:])], reads=[x1_], writes=[self.SB["x1"]])

    def phase_moe(self):
        k, I, S = self.k, self.I, self.S
        identb = self.load_const("identb")
        tri = self.load_const("tri")
        onesb = self.load_const("onesb")
        iota = self.load_const("iota")
        xacc = k.sb("xacc", [128, 8, D], F32)
        xab = [Buf("xacc%d" % i) for i in range(8)]
        maskf = k.sb("maskf", [128, 8, NE], F32)
        maskb = k.sb("maskb", [128, 8, NE], BF16)
        gate = k.sb("gate", [128, 8, NE], F32)
        pos = k.sb("pos", [128, 8, NE], F32)
        k.op("pool", lambda e: e.memset(xacc.t[:], 0.0), writes=xab)
        with k.scope():
            identf = self.load_const("identf")
            gam2b = self.bvec(3, "gam2b")
            sh2b = self.bvec(4, "sh2b", q="act")
            wr = k.sb("wr", [128, 16, NE], F32)
            k.dma("sp", [(wr.t[:], I["w_router"].rearrange("(k p) e -> p k e", p=128))], writes=[wr])
            brb = k.sb("brb", [128, NE], F32)
            k.dma("sp", [(brb.t[:], I["b_router"].partition_broadcast(128))], writes=[brb])
            xt = [k.sb("mxt%d" % i, [128, D], F32) for i in range(2)]
            junk = k.sb("mjunk", [128, D], BF16)
            tmp = k.sb("mtmp", [128, D], F32)
            h2f = [k.sb("h2f%d" % i, [128, D], F32) for i in range(2)]
            h2bf = [k.sb("h2bf%d" % i, [128, D], BF16) for i in range(2)]
            ms = [k.sb("mms%d" % i, [128, 4], F32) for i in range(2)]
            hTf = k.sb("hTf", [128, 16, 128], F32)
            ptrf = [k.ps("ptrf%d" % i, [128, 512]) for i in range(2)]
            plg = k.ps("plg", [128, NE])
            ppos = k.ps("ppos", [128, NE])
            lg = k.sb("lg", [128, NE], F32)
            m8 = k.sb("rm8", [128, 8], F32)
            sm = k.sb("rsm", [128, 4], F32)
            ex = k.sb("rex", [128, NE], F32)
            for qt in range(8):
                x_, h_ = xt[qt % 2], h2f[qt % 2]
                k.dma("sp", [(x_.t[:], S["x1"][qt * 128:(qt + 1) * 128, :])], reads=[self.SB["x1"]], writes=[x_])
                self.norm_tile(x_, gam2b, sh2b, h_, ms[qt % 2], junk, tmp)
                hb_ = h2bf[qt % 2]
                k.op("act", lambda e, h_=h_, hb_=hb_: e.copy(hb_.t[:], h_.t[:]), reads=[h_], writes=[hb_])
                k.dma("act", [(S["h2s"][:, :, qt, :].rearrange("k p f -> p k f"), hb_.t[:].rearrange("p (k f) -> p k f", f=128))],
                      reads=[hb_], writes=[self.SB["h2s"]])
                for rnd in range(4):
                    p = ptrf[rnd % 2]
                    for kk in range(4):
                        kc = rnd * 4 + kk
                        k.op("pe", lambda e, p=p, kk=kk, kc=kc, h_=h_: e.transpose(
                            p.t[:, kk * 128:(kk + 1) * 128], h_.t[:, kc * 128:(kc + 1) * 128], identf.t[:]),
                            reads=[h_, identf], writes=[p])
                    dst = hTf.t[:, rnd * 4:(rnd + 1) * 4, :]
                    src = p.t[:].rearrange("p (a b) -> p a b", a=4)
                    k.op("dve", lambda e, dst=dst, src=src: e.tensor_copy(dst, src), reads=[p], writes=[hTf])
                for kk in range(16):
                    k.op("pe", lambda e, kk=kk: e.matmul(plg.t[:], hTf.t[:, kk, :], wr.t[:, kk, :], start=(kk == 0), stop=(kk == 15)),
                         reads=[hTf, wr], writes=[plg])
                k.op("dve", lambda e: e.tensor_tensor(lg.t[:], plg.t[:], brb.t[:], ALU.add), reads=[plg, brb], writes=[lg])
                k.op("dve", lambda e: e.max(m8.t[:], lg.t[:]), reads=[lg], writes=[m8])
                k.op("dve", lambda e, qt=qt: e.tensor_scalar(maskf.t[:, qt, :], lg.t[:], m8.t[:, 3:4], None, op0=ALU.is_ge),
                     reads=[lg, m8], writes=[maskf])
                k.op("pool", lambda e, qt=qt: e.tensor_copy(maskb.t[:, qt, :], maskf.t[:, qt, :]), reads=[maskf], writes=[maskb])
                k.op("dve", lambda e: e.tensor_scalar(sm.t[:, 0:1], m8.t[:, 0:1], -1.0, None, op0=ALU.mult),
                     reads=[m8], writes=[sm])
                k.op("act", lambda e: e.activation(ex.t[:], lg.t[:], AF.Exp, bias=sm.t[:, 0:1], scale=1.0),
                     reads=[lg, sm], writes=[ex])
                k.op("dve", lambda e, qt=qt: e.tensor_tensor(ex.t[:], ex.t[:], maskf.t[:, qt, :], ALU.mult),
                     reads=[ex, maskf], writes=[ex])
                k.op("dve", lambda e: e.reduce_sum(sm.t[:, 1:2], ex.t[:], AX.X), reads=[ex], writes=[sm])
                k.op("dve", lambda e: e.reciprocal(sm.t[:, 2:3], sm.t[:, 1:2]), reads=[sm], writes=[sm])
                k.op("dve", lambda e, qt=qt: e.tensor_scalar(gate.t[:, qt, :], ex.t[:], sm.t[:, 2:3], None, op0=ALU.mult),
                     reads=[ex, sm], writes=[gate])
                k.op("pe", lambda e, qt=qt: e.matmul(ppos.t[:], tri.t[:], maskb.t[:, qt, :], start=True, stop=(qt == 0)),
                     reads=[tri, maskb], writes=[ppos])
                for i2 in range(qt):
                    k.op("pe", lambda e, i2=i2, qt=qt: e.matmul(ppos.t[:], onesb.t[:], maskb.t[:, i2, :], start=False,
                                                                stop=(i2 == qt - 1)), reads=[onesb, maskb], writes=[ppos])
                k.op("act", lambda e, qt=qt: e.copy(pos.t[:, qt, :], ppos.t[:]), reads=[ppos], writes=[pos])
        with k.scope():
            NST = CAP // 128
            Sm = k.sb("Smat", [128, 8, CAP], BF16)
            SGq = [k.sb("SGq%d" % i, [128, CAP], BF16) for i in range(2)]
            hk = [k.sb("hk%d" % i, [128, 8, 128], BF16) for i in range(2)]
            XeT = k.sb("XeT", [128, 16, CAP], BF16)
            actT = k.sb("actT", [128, 16, CAP], BF16)
            Yec = [k.sb("Yec%d" % i, [128, NST, 512], BF16) for i in range(2)]
            SGT = k.sb("SGT", [128, NST, NQ], BF16)
            b1t = [k.sb("b1t%d" % i, [128, 32], F32) for i in range(2)]
            wst = [k.sb("mwst%d" % i, [128, 16, 256], F32) for i in range(2)]
            wbf = [k.sb("mwbf%d" % i, [128, 16, 256], BF16) for i in range(2)]
            gl = [k.sb("gl%d" % i, [128, CAP], F32) for i in range(2)]
            ln_ = [k.sb("ln%d" % i, [128, CAP], F32) for i in range(2)]
            sg_ = [k.sb("sg%d" % i, [128, CAP], F32) for i in range(2)]
            xps = k.ps("xps", [128, CAP])
            pgl = [k.ps("pgl%d" % i, [128, 2, CAP]) for i in range(2)]
            py = k.ps("py", [128, 2, 256])
            psg = k.ps("psg", [128, 1024], BF16)
            psc = k.ps("psc", [128, 512])
            wc = [0]
            un = [0]
            hc = [0]

            def load_w(src_ap):
                i = wc[0] % 2
                wc[0] += 1
                ws, wb = wst[i], wbf[i]
                k.dma("sp", [(ws.t[:, 4 * j:4 * j + 4, :], src_ap[:, 4 * j:4 * j + 4, :]) for j in range(4)], writes=[ws])
                if wc[0] % 2 == 0:
                    k.op("pool", lambda e: e.tensor_copy(wb.t[:], ws.t[:]), reads=[ws], writes=[wb])
                else:
                    k.op("act", lambda e: e.copy(wb.t[:], ws.t[:]), reads=[ws], writes=[wb])
                return wb

            for ex_ in range(NE):
                b1 = b1t[ex_ % 2]
                k.dma("act", [(b1.t[:], I["b_exp1"][ex_].rearrange("(c p) -> p c", p=128))], writes=[b1],
                      allow_slow_non_contiguous=True)
                for qt in range(8):
                    k.op("dve", lambda e, qt=qt: e.tensor_scalar(Sm.t[:, qt, :], iota.t[:], pos.t[:, qt, ex_:ex_ + 1],
                                                                 maskf.t[:, qt, ex_:ex_ + 1], op0=ALU.is_equal, op1=ALU.mult),
                         reads=[iota, pos, maskf], writes=[Sm])
                for fk in range(16):
                    h_ = hk[hc[0] % 2]
                    hc[0] += 1
                    k.dma("pool", [(h_.t[:], S["h2s"][fk])], reads=[self.SB["h2s"]], writes=[h_])
                    for qt in range(8):
                        k.op("pe", lambda e, h_=h_, qt=qt: e.matmul(xps.t[:], h_.t[:, qt, :], Sm.t[:, qt, :],
                                                                    start=(qt == 0), stop=(qt == 7)),
                             reads=[h_, Sm], writes=[xps])
                    k.op("act", lambda e, fk=fk: e.copy(XeT.t[:, fk, :], xps.t[:]), reads=[xps], writes=[XeT])
                for qt in range(8):
                    q_ = SGq[qt % 2]
                    k.op("pool", lambda e, qt=qt, q_=q_: e.tensor_scalar(q_.t[:], iota.t[:], pos.t[:, qt, ex_:ex_ + 1],
                                                                        gate.t[:, qt, ex_:ex_ + 1], op0=ALU.is_equal, op1=ALU.mult),
                         reads=[iota, pos, gate], writes=[q_])
                    hb = qt % 2
                    for s_ in range(NST):
                        k.op("pe", lambda e, s_=s_, q_=q_, hb=hb: e.transpose(
                            psg.t[:, hb * 512 + s_ * 128:hb * 512 + (s_ + 1) * 128], q_.t[:, s_ * 128:(s_ + 1) * 128], identb.t[:]),
                            reads=[q_, identb], writes=[psg])
                    k.op("act", lambda e, qt=qt, hb=hb: e.copy(
                        SGT.t[:, :, qt * 128:(qt + 1) * 128], psg.t[:, hb * 512:(hb + 1) * 512].rearrange("p (a b) -> p a b", a=NST)),
                        reads=[psg], writes=[SGT])
                for cc in range(8):
                    wg = load_w(I["w_exp1"][ex_][:, cc * 256:(cc + 1) * 256].rearrange("(k p) n -> p k n", p=128))
                    wl = load_w(I["w_exp1"][ex_][:, DFF + cc * 256:DFF + (cc + 1) * 256].rearrange("(k p) n -> p k n", p=128))
                    for u in range(2):
                        f = cc * 2 + u
                        b = un[0] % 2
                        un[0] += 1
                        pg = pgl[b]
                        for kk in range(16):
                            k.op("pe", lambda e, kk=kk, u=u, pg=pg, wg=wg: e.matmul(
                                pg.t[:, 0, :], wg.t[:, kk, u * 128:(u + 1) * 128], XeT.t[:, kk, :],
                                start=(kk == 0), stop=(kk == 15)), reads=[wg, XeT], writes=[pg])
                        for kk in range(16):
                            k.op("pe", lambda e, kk=kk, u=u, pg=pg, wl=wl: e.matmul(
                                pg.t[:, 1, :], wl.t[:, kk, u * 128:(u + 1) * 128], XeT.t[:, kk, :],
                                start=(kk == 0), stop=(kk == 15)), reads=[wl, XeT], writes=[pg])
                        k.op("dve", lambda e, b=b, pg=pg, f=f: e.tensor_scalar(gl[b].t[:], pg.t[:, 0, :], b1.t[:, f:f + 1], 7.0,
                                                                              op0=ALU.add, op1=ALU.min),
                             reads=[pg, b1], writes=[gl[b]])
                        k.op("dve", lambda e, b=b, pg=pg, f=f: e.tensor_scalar(ln_[b].t[:], pg.t[:, 1, :], b1.t[:, 16 + f:17 + f], -7.0,
                                                                              op0=ALU.add, op1=ALU.max),
                             reads=[pg, b1], writes=[ln_[b]])
                        k.op("pool", lambda e, b=b: e.tensor_scalar(ln_[b].t[:], ln_[b].t[:], 7.0, 1.0, op0=ALU.min, op1=ALU.add),
                             reads=[ln_[b]], writes=[ln_[b]])
                        k.op("act", lambda e, b=b: e.activation(sg_[b].t[:], gl[b].t[:], AF.Sigmoid, scale=1.702),
                             reads=[gl[b]], writes=[sg_[b]])
                        k.op("pool", lambda e, b=b: e.tensor_tensor(sg_[b].t[:], gl[b].t[:], sg_[b].t[:], ALU.mult),
                             reads=[gl[b], sg_[b]], writes=[sg_[b]])
                        k.op("dve", lambda e, b=b, f=f: e.tensor_tensor(actT.t[:, f, :], sg_[b].t[:], ln_[b].t[:], ALU.mult),
                             reads=[sg_[b], ln_[b]], writes=[actT])
                for c in range(4):
                    ye = Yec[c % 2]
                    for half in range(2):
                        c0 = c * 512 + half * 256
                        w2 = load_w(I["w_exp2"][ex_][:, c0:c0 + 256].rearrange("(k p) n -> p k n", p=128))
                        for s_ in range(NST):
                            for fk in range(16):
                                k.op("pe", lambda e, fk=fk, s_=s_, w2=w2: e.matmul(
                                    py.t[:, s_ % 2, :], actT.t[:, fk, s_ * 128:(s_ + 1) * 128], w2.t[:, fk, :],
                                    start=(fk == 0), stop=(fk == 15)), reads=[actT, w2], writes=[py])
                            k.op("act", lambda e, s_=s_, half=half, ye=ye: e.copy(ye.t[:, s_, half * 256:(half + 1) * 256],
                                                                               py.t[:, s_ % 2, :]), reads=[py], writes=[ye])
                    for qt in range(8):
                        for s_ in range(NST):
                            k.op("pe", lambda e, s_=s_, qt=qt, ye=ye: e.matmul(
                                psc.t[:], SGT.t[:, s_, qt * 128:(qt + 1) * 128], ye.t[:, s_, :],
                                start=(s_ == 0), stop=(s_ == NST - 1)), reads=[SGT, ye], writes=[psc])
                        k.op("dve", lambda e, qt=qt, c=c: e.tensor_tensor(
                            xacc.t[:, qt, c * 512:(c + 1) * 512], xacc.t[:, qt, c * 512:(c + 1) * 512], psc.t[:], ALU.add),
                            reads=[psc, xab[qt]], writes=[xab[qt]])
        with k.scope():
            identf = self.load_const("identf")
            b2all = k.sb("b2all", [NE, D], F32)
            k.dma("sp", [(b2all.t[:], I["b_exp2"])], writes=[b2all])
            g2b = self.bvec(5, "g2b")
            gT = [k.sb("gT%d" % i, [NE, 128], F32) for i in range(2)]
            pgt = k.ps("pgt", [NE, 128])
            pb2 = [k.ps("pb2_%d" % i, [128, 512]) for i in range(2)]
            xt = [k.sb("fxt%d" % i, [128, D], F32) for i in range(2)]
            x2 = [k.sb("fx2_%d" % i, [128, D], F32) for i in range(2)]
            n = 0
            if self.last:
                nfb = k.sb("nfb", [128, D], F32)
                k.dma("sp", [(nfb.t[:], I["norm_final"].partition_broadcast(128))], writes=[nfb])
                junk = k.sb("fjunk", [128, D], BF16)
                ms = [k.sb("fms%d" % i, [128, 4], F32) for i in range(2)]
                yo = [k.sb("fyo%d" % i, [128, D], F32) for i in range(2)]
            for qt in range(8):
                g_ = gT[qt % 2]
                k.op("pe", lambda e, qt=qt: e.transpose(pgt.t[:], gate.t[:, qt, :], identf.t[:]), reads=[gate, identf], writes=[pgt])
                k.op("act", lambda e, g_=g_: e.copy(g_.t[:], pgt.t[:]), reads=[pgt], writes=[g_])
                x_, x2_ = xt[qt % 2], x2[qt % 2]
                k.dma("sp", [(x_.t[:], S["x1"][qt * 128:(qt + 1) * 128, :])], reads=[self.SB["x1"]], writes=[x_])
                for c in range(4):
                    p = pb2[n % 2]
                    n += 1
                    k.op("pe", lambda e, p=p, c=c, g_=g_: e.matmul(p.t[:], g_.t[:], b2all.t[:, c * 512:(c + 1) * 512],
                                                                   start=True, stop=True), reads=[g_, b2all], writes=[p])
                    k.op("dve", lambda e, p=p, qt=qt, c=c: e.tensor_tensor(
                        xacc.t[:, qt, c * 512:(c + 1) * 512], xacc.t[:, qt, c * 512:(c + 1) * 512], p.t[:], ALU.add),
                        reads=[p, xab[qt]], writes=[xab[qt]])
                k.op("pool", lambda e, qt=qt: e.tensor_tensor(xacc.t[:, qt, :], xacc.t[:, qt, :], g2b.t[:], ALU.mult),
                     reads=[xab[qt], g2b], writes=[xab[qt]])
                k.op("dve", lambda e, qt=qt, x_=x_, x2_=x2_: e.tensor_tensor(x2_.t[:], xacc.t[:, qt, :], x_.t[:], ALU.add),
                     reads=[xab[qt], x_], writes=[x2_])
                if self.last:
                    m_, y_ = ms[qt % 2], yo[qt % 2]
                    k.op("pool", lambda e, m_=m_: e.memset(m_.t[:, 0:1], 0.0), writes=[m_])
                    k.op("act", lambda e, m_=m_, x2_=x2_: e.activation(junk.t[:], x2_.t[:], AF.Square, scale=float(D ** -0.5),
                                                                       accum_out=m_.t[:, 0:1]), reads=[x2_, m_], writes=[junk, m_])
                    k.op("act", lambda e, m_=m_: e.activation(m_.t[:, 1:2], m_.t[:, 0:1], AF.Sqrt, bias=EPS, scale=1.0),
                         reads=[m_], writes=[m_])
                    k.op("dve", lambda e, m_=m_: e.reciprocal(m_.t[:, 2:3], m_.t[:, 1:2]), reads=[m_], writes=[m_])
                    k.op("dve", lambda e, m_=m_, y_=y_, x2_=x2_: e.scalar_tensor_tensor(
                        y_.t[:], x2_.t[:], m_.t[:, 2:3], nfb.t[:], ALU.mult, ALU.mult), reads=[x2_, m_, nfb], writes=[y_])
                    k.dma("act", [(self.out[qt * 128:(qt + 1) * 128, :], y_.t[:])], reads=[y_], writes=[self.outB])
                else:
                    k.dma("act", [(self.out[qt * 128:(qt + 1) * 128, :], x2_.t[:])], reads=[x2_], writes=[self.outB])


_PROG_CACHE = {}


def _get_prog(last):
    if last not in _PROG_CACHE:
        p = Prog(last)
        p.build()
        _PROG_CACHE[last] = p
    return _PROG_CACHE[last]


def kernel(**inputs):
    x = np.ascontiguousarray(np.asarray(inputs["x"], dtype=np.float32))
    B = x.shape[0]
    sc = shared_consts()
    ccs = [core_consts(r) for r in range(4)]
    for layer in range(2):
        prog = _get_prog(layer == 1)
        maps = []
        for c in range(8):
            b, r = c // 4, c % 4
            pad = 1024 * (3 - r)
            m = {}
            for n in prog.I.keys():
                if n == "xin":
                    xl = np.zeros((NTOK, D), np.float32)
                    xl[pad:TL] = x[b, :TL - pad]
                    xl[TL:] = x[b, 1024 * r:1024 * (r + 1)]
                    m[n] = xl
                elif n == "cvec":
                    m[n] = np.ascontiguousarray(np.asarray(inputs["c"], dtype=np.float32)[b])
                elif n == "norm_final":
                    m[n] = np.ascontiguousarray(np.asarray(inputs[n], dtype=np.float32))
                elif n in WEIGHT_SPECS:
                    m[n] = np.ascontiguousarray(np.asarray(inputs[n], dtype=np.float32)[layer])
                elif n in sc:
                    m[n] = sc[n]
                else:
                    m[n] = ccs[r][n]
            maps.append(m)
        res = run_bass_kernel_spmd(prog.nc, maps, core_ids=list(range(8)))
        xn = np.empty_like(x)
        for c in range(8):
            b, r = c // 4, c % 4
            xn[b, 1024 * r:1024 * (r + 1)] = res.results[c]["out"]
        x = xn
    return x
```

```python
import contextlib
import numpy as np
import ml_dtypes
import concourse.bass as bass
import concourse.mybir as mybir
from concourse.bass_utils import run_bass_kernel_spmd

F32 = mybir.dt.float32
BF16 = mybir.dt.bfloat16
ALU = mybir.AluOpType
AF = mybir.ActivationFunctionType
AX = mybir.AxisListType
NPBF = ml_dtypes.bfloat16

D = 2048
TL = 4096
NQ = 1024
QOFF = TL - NQ
NT = TL // 128
NTOK = TL + NQ
INW = 5796
CAP = 512
NE = 32
DFF = 2048
EPS = 1e-6
SCALE = 0.125
NDS = 40


class Buf:
    __slots__ = ("name", "w", "r")

    def __init__(self, name=""):
        self.name = name
        self.w = None
        self.r = {}


class Tl:
    __slots__ = ("t", "b")

    def __init__(self, t, b):
        self.t = t
        self.b = b


class KB:
    def __init__(self, nc, es):
        self.nc = nc
        self.es = es
        self.engs = {"pe": nc.tensor, "act": nc.scalar, "dve": nc.vector,
                     "pool": nc.gpsimd, "sp": nc.sync}
        self.csem = {e: es.enter_context(nc.semaphore("c_" + e)) for e in ("pe", "act", "dve", "pool")}
        self.ccnt = {e: 0 for e in self.csem}
        self.dsems = [es.enter_context(nc.semaphore("dq%d" % i)) for i in range(NDS)]
        self.dcnt = [0] * NDS
        self.dnext = 0
        self.known = {e: {} for e in self.engs}
        self.n = 0

    @contextlib.contextmanager
    def scope(self):
        old = self.es
        with contextlib.ExitStack() as es:
            self.es = es
            try:
                yield
            finally:
                self.barrier()
                self.es = old

    def sb(self, name, shape, dt):
        self.n += 1
        t = self.es.enter_context(self.nc.sbuf_tensor("%s_%d" % (name, self.n), list(shape), dt))
        return Tl(t, Buf(name))

    def ps(self, name, shape, dt=F32):
        self.n += 1
        t = self.es.enter_context(self.nc.psum_tensor("%s_%d" % (name, self.n), list(shape), dt))
        return Tl(t, Buf(name))

    def _wait(self, eng, tok):
        key, sem, val = tok
        if self.known[eng].get(key, 0) >= val:
            return
        self.engs[eng].wait_ge(sem, val)
        self.known[eng][key] = val

    def _deps(self, eng, reads, writes):
        toks = []
        for b in reads:
            if b.w is not None:
                toks.append(b.w)
        for b in writes:
            if b.w is not None and b.w[0] != eng:
                toks.append(b.w)
            for kk, t in b.r.items():
                if kk != eng:
                    toks.append(t)
        for t in toks:
            if eng == "pe" and t[0] == "pe":
                continue
            self._wait(eng, t)

    def _mark(self, tok, reads, writes):
        for b in reads:
            old = b.r.get(tok[0])
            if old is None or old[2] < tok[2]:
                b.r[tok[0]] = tok
        for b in writes:
            b.w = tok
            b.r = {}

    def op(self, eng, fn, reads=(), writes=()):
        reads = [x.b if isinstance(x, Tl) else x for x in reads]
        writes = [x.b if isinstance(x, Tl) else x for x in writes]
        self._deps(eng, reads, writes)
        ins = fn(self.engs[eng])
        self.ccnt[eng] += 1
        ins.then_inc(self.csem[eng], 1)
        tok = (eng, self.csem[eng], self.ccnt[eng])
        self._mark(tok, reads, writes)
        return tok

    def dma(self, q, pairs, reads=(), writes=(), **kw):
        reads = [x.b if isinstance(x, Tl) else x for x in reads]
        writes = [x.b if isinstance(x, Tl) else x for x in writes]
        self._deps(q, reads, writes)
        i = self.dnext
        self.dnext = (self.dnext + 1) % NDS
        sem = self.dsems[i]
        key = "d%d" % i
        if self.dcnt[i] > 0:
            self._wait(q, (key, sem, self.dcnt[i]))
        for (o, a) in pairs:
            self.engs[q].dma_start(out=o, in_=a, **kw).then_inc(sem, 16)
            self.dcnt[i] += 16
        tok = (key, sem, self.dcnt[i])
        self._mark(tok, reads, writes)
        return tok

    def _alltoks(self):
        toks = [(e, self.csem[e], self.ccnt[e]) for e in self.csem if self.ccnt[e] > 0]
        toks += [("d%d" % i, self.dsems[i], self.dcnt[i]) for i in range(NDS) if self.dcnt[i] > 0]
        return toks

    def barrier(self):
        toks = self._alltoks()
        for e in self.engs:
            for t in toks:
                if t[0] != e:
                    self._wait(e, t)

    def finish(self, eng="sp"):
        for t in self._alltoks():
            self._wait(eng, t)


def _rope_rows(pos, nrows):
    inv = (1.0 / (10000.0 ** (np.arange(0, 64, 2, dtype=np.float32) / np.float32(64)))).astype(np.float32)
    ang = pos.astype(np.float32)[None, :] * inv[:, None]
    cos, sin = np.cos(ang).astype(np.float32), np.sin(ang).astype(np.float32)
    p = np.arange(nrows)
    c2 = cos[p % 32]
    sgn = np.where((p % 64) < 32, -1.0, 1.0).astype(np.float32)
    s2 = sin[p % 32] * sgn[:, None]
    return np.stack([c2, s2]).astype(np.float32)


def shared_consts():
    c = {}
    q = np.arange(512)[None, :]
    s = np.arange(128)[:, None]
    mA = np.zeros((20, 128, 512), np.float32)
    for idx in range(20):
        d = 128 * (idx - 3) + (q - s)
        m = ((d >= 0) & (d <= 128)).astype(np.float32)
        m += ((d >= 0) & (d <= 512) & (d % 4 == 0))
        m += ((d >= 0) & (d <= 2048) & (d % 16 == 0))
        mA[idx] = m
    c["maskA"] = mA.astype(NPBF)
    mC = np.zeros((4, 128, 512), np.float32)
    mS = np.zeros((4, 128, 512), np.float32)
    for dl in range(4):
        sa = 128 * dl + s
        mC[dl] = (sa <= q)
        mS[dl] = (sa < q)
    c["maskCaus"] = mC.astype(NPBF)
    c["maskStrictF"] = mS.astype(np.float32)
    c["maskStrictB"] = mS.astype(NPBF)
    mW = np.zeros((8, 128, 512), np.float32)
    for i in range(8):
        d = q - (128 * (i - 4) + s)
        mW[i] = (d >= 0) & (d < 512)
    c["maskW"] = mW.astype(NPBF)
    mCmp = np.zeros((2, 2, 128, 512), np.float32)
    for qg in range(2):
        for nt in range(2):
            n = 128 * nt + s
            t = QOFF + 512 * qg + q
            mCmp[qg, nt] = (16 * n + 31 <= t) & (n <= 254)
    c["maskCmp"] = mCmp.astype(NPBF)
    ss = np.arange(TL)
    c["expand"] = (ss[None, :] // 64 == np.arange(64)[:, None]).astype(NPBF)
    j = np.arange(128)
    c["u2"] = (j[:, None] > j[None, :]).astype(np.float32)
    c["onesf"] = np.ones((128, 128), np.float32)
    c["tri"] = (j[:, None] < j[None, :]).astype(NPBF)
    c["onesb"] = np.ones((128, 128), NPBF)
    c["identf"] = np.eye(128, dtype=np.float32)
    c["identb"] = np.eye(128).astype(NPBF)
    sw = np.zeros((128, 128), np.float32)
    for m in range(128):
        sw[(m + 32) % 64 + 64 * (m // 64), m] = 1.0
    c["pswap"] = sw
    c["iota"] = np.tile(np.arange(CAP, dtype=np.float32)[None, :], (128, 1))
    return c


def core_consts(r):
    pad = 1024 * (3 - r)
    c = {}
    posl = np.arange(TL) - pad
    pos_all = np.concatenate([posl, posl[QOFF:]])
    c["rope"] = _rope_rows(pos_all, 128)
    n = np.arange(256)
    c["ropec"] = _rope_rows(16 * n + 31 - pad, 64)
    tl = np.arange(TL).reshape(NT, 128).T
    c["kvalid"] = (tl >= pad).astype(np.float32)
    nn = np.arange(256).reshape(2, 128).T
    cv = ((nn <= 254) & (16 * nn >= pad)).astype(np.float32)
    c["cvalid"] = cv
    m = np.arange(64)
    cs = 16 * np.arange(256)[:, None]
    sm = 64 * m[None, :]
    ov = ((cs < sm + 64) & (cs + 32 > sm)).astype(np.float32)
    ov = ov.reshape(2, 128, 64).transpose(1, 0, 2) * cv[:, :, None]
    c["ovl"] = ov.astype(NPBF)
    t = QOFF + np.arange(NQ)[:, None]
    tb = t // 64
    m0 = pad // 64
    mm = m[None, :]
    bias = np.zeros((NQ, 64), np.float32)
    forced = (mm == m0) | (mm == tb) | (mm == tb - 1)
    bias[forced] = 1e4
    bias[np.broadcast_to(mm > tb, bias.shape)] = -1e4
    bias[np.broadcast_to(mm < m0, bias.shape)] = -1e4
    c["impbias"] = bias
    return c


CONST_SPECS = {
    "maskA": ([20, 128, 512], BF16), "maskCaus": ([4, 128, 512], BF16),
    "maskStrictF": ([4, 128, 512], F32), "maskStrictB": ([4, 128, 512], BF16),
    "maskW": ([8, 128, 512], BF16), "maskCmp": ([2, 2, 128, 512], BF16),
    "expand": ([64, TL], BF16), "u2": ([128, 128], F32), "onesf": ([128, 128], F32),
    "tri": ([128, 128], BF16), "onesb": ([128, 128], BF16), "identf": ([128, 128], F32),
    "identb": ([128, 128], BF16), "pswap": ([128, 128], F32), "iota": ([128, CAP], F32),
    "rope": ([2, 128, NTOK], F32), "ropec": ([2, 64, 256], F32), "kvalid": ([128, NT], F32),
    "cvalid": ([128, 2], F32), "ovl": ([128, 2, 64], BF16), "impbias": ([NQ, 64], F32),
}

WEIGHT_SPECS = {
    "w_mod": [D, 6 * D], "b_mod": [6 * D], "norm_attn": [D], "norm_ffn": [D],
    "w_in": [D, INW], "cmp_pe": [2, 32, 64], "cmp_w1": [2, 2048, 128], "cmp_w2": [2, 128, 64],
    "mix_norm": [D], "w_out": [D, D], "w_router": [D, NE], "b_router": [NE],
    "w_exp1": [NE, D, 2 * DFF], "b_exp1": [NE, 2 * DFF], "w_exp2": [NE, DFF, D], "b_exp2": [NE, D],
    "norm_final": [D],
}

SCRATCH = {
    "vecs": ([6, D], F32), "kTA": ([768, TL], BF16), "vA": ([TL, 768], BF16), "cT": ([384, TL], BF16),
    "kTS": ([192, TL], BF16), "kTW": ([192, TL], BF16), "vS": ([TL, 192], BF16), "vW": ([TL, 192], BF16),
    "kTC": ([512, TL], BF16), "vC": ([TL, 512], BF16), "qTA": ([768, NQ], BF16), "qTB": ([768, NQ], BF16),
    "qTC": ([512, NQ], BF16), "gates": ([NQ, 36], F32), "otok": ([NQ, D], BF16), "x1": ([NQ, D], F32), "h2s": ([16, 128, 8, 128], BF16),
}


class Prog:
    def __init__(self, last, debug=(), phases=None):
        self.last = last
        self.debug = set(debug)
        self.phases = phases
        nc = bass.Bass("TRN2", target_bir_lowering=False)
        self.nc = nc
        specs = {"xin": ([NTOK, D], F32), "cvec": ([D], F32)}
        specs.update({n: (shp, F32) for n, shp in WEIGHT_SPECS.items()})
        specs.update(CONST_SPECS)

        class Lazy(dict):
            def __missing__(d, n):
                shp, dt = specs[n]
                d[n] = nc.dram_tensor(n, shp, dt, kind="ExternalInput").ap()
                return d[n]
        self.I = Lazy()
        self.S = {}
        for n, (shp, dt) in SCRATCH.items():
            kind = "ExternalOutput" if n in self.debug else "Internal"
            self.S[n] = nc.dram_tensor("s_" + n, shp, dt, kind=kind).ap()
        self.SB = {n: Buf("s_" + n) for n in SCRATCH}
        self.out = nc.dram_tensor("out", [NQ, D], F32, kind="ExternalOutput").ap()
        self.outB = Buf("out")

    def run(self, what):
        return self.phases is None or what in self.phases

    def build(self):
        with contextlib.ExitStack() as es:
            k = KB(self.nc, es)
            self.k = k
            if self.run("mod"):
                with k.scope():
                    self.phase_mod()
            if self.run("proj"):
                with k.scope():
                    self.phase_proj()
            if self.run("attA"):
                with k.scope():
                    self.phase_att_a()
            if self.run("attB"):
                with k.scope():
                    self.phase_att_b()
            if self.run("attC"):
                with k.scope():
                    self.phase_att_c()
            if self.run("oproj"):
                with k.scope():
                    self.phase_oproj()
            if self.run("moe"):
                with k.scope():
                    self.phase_moe()
            k.finish("sp")
        return self.nc

    def load_const(self, name, q="sp", rearr=None, sl=None):
        k = self.k
        shp, dt = CONST_SPECS[name]
        src = self.I[name]
        if rearr is not None:
            src = src.rearrange(rearr)
        t = k.sb(name, list(src.shape), dt)
        k.dma(q, [(t.t[:], src)], writes=[t])
        return t

    def phase_mod(self):
        k, I = self.k, self.I
        cT = k.sb("cT", [128, 16], F32)
        k.dma("sp", [(cT.t[:], I["cvec"].rearrange("(k p) -> p k", p=128))], writes=[cT],
              allow_slow_non_contiguous=True)
        cact = k.sb("cact", [128, 16], F32)
        k.op("act", lambda e: e.activation(cact.t[:], cT.t[:], AF.Silu), reads=[cT], writes=[cact])
        modrow = k.sb("modrow", [1, 6 * D], F32)
        k.dma("sp", [(modrow.t[:], I["b_mod"].rearrange("(o n) -> o n", o=1))], writes=[modrow])
        nrow = k.sb("nrow", [1, 2, D], F32)
        k.dma("sp", [(nrow.t[:, 0, :], I["norm_attn"].rearrange("(o n) -> o n", o=1)),
                     (nrow.t[:, 1, :], I["norm_ffn"].rearrange("(o n) -> o n", o=1))], writes=[nrow])
        wt = [k.sb("wmod%d" % i, [128, 16, 512], F32) for i in range(2)]
        pm = [k.ps("pm%d" % i, [1, 512]) for i in range(2)]
        wsrc = I["w_mod"]
        for n in range(24):
            w = wt[n % 2]
            src = wsrc[:, n * 512:(n + 1) * 512].rearrange("(k p) n -> p k n", p=128)
            k.dma("sp", [(w.t[:, 4 * i:4 * i + 4, :], src[:, 4 * i:4 * i + 4, :]) for i in range(4)], writes=[w])
            p = pm[n % 2]
            for kk in range(16):
                k.op("pe", lambda e, kk=kk, w=w, p=p: e.matmul(p.t[:], cact.t[:, kk:kk + 1], w.t[:, kk, :],
                                                             start=(kk == 0), stop=(kk == 15)),
                     reads=[cact, w], writes=[p])
            k.op("dve", lambda e, n=n, p=p: e.tensor_tensor(modrow.t[:, n * 512:(n + 1) * 512], p.t[:],
                                                          modrow.t[:, n * 512:(n + 1) * 512], ALU.add),
                 reads=[p, modrow], writes=[modrow])
        m = lambda i: modrow.t[:, i * D:(i + 1) * D]
        k.op("dve", lambda e: e.scalar_tensor_tensor(m(1), m(1), 1.0, nrow.t[:, 0, :], ALU.add, ALU.mult),
             reads=[modrow, nrow], writes=[modrow])
        k.op("dve", lambda e: e.scalar_tensor_tensor(m(4), m(4), 1.0, nrow.t[:, 1, :], ALU.add, ALU.mult),
             reads=[modrow, nrow], writes=[modrow])
        order = [1, 0, 2, 4, 3, 5]
        k.dma("sp", [(self.S["vecs"][i:i + 1, :], m(sg)) for i, sg in enumerate(order)], reads=[modrow],
              writes=[self.SB["vecs"]])

    def bvec(self, idx, name, q="sp"):
        k = self.k
        t = k.sb(name, [128, D], F32)
        k.dma(q, [(t.t[:], self.S["vecs"][idx, :].partition_broadcast(128))], reads=[self.SB["vecs"]], writes=[t])
        return t

    def norm_tile(self, xt, gamb, shb, hb, ms, junk, tmp):
        k = self.k
        k.op("pool", lambda e: e.memset(ms.t[:, 0:1], 0.0), writes=[ms])
        k.op("act", lambda e: e.activation(junk.t[:], xt.t[:], AF.Square, scale=float(D ** -0.5),
                                           accum_out=ms.t[:, 0:1]), reads=[xt, ms], writes=[junk, ms])
        k.op("act", lambda e: e.activation(ms.t[:, 1:2], ms.t[:, 0:1], AF.Sqrt, bias=EPS, scale=1.0),
             reads=[ms], writes=[ms])
        k.op("dve", lambda e: e.reciprocal(ms.t[:, 2:3], ms.t[:, 1:2]), reads=[ms], writes=[ms])
        k.op("dve", lambda e: e.scalar_tensor_tensor(tmp.t[:], xt.t[:], ms.t[:, 2:3], gamb.t[:], ALU.mult, ALU.mult),
             reads=[xt, ms, gamb], writes=[tmp])
        k.op("pool", lambda e: e.tensor_tensor(hb.t[:], tmp.t[:], shb.t[:], ALU.add), reads=[tmp, shb], writes=[hb])

    def phase_proj(self):
        k, I, S = self.k, self.I, self.S
        gamb = self.bvec(0, "gam1b")
        shb = self.bvec(1, "sh1b", q="act")
        identb = self.load_const("identb")
        pswap = self.load_const("pswap")
        kval = self.load_const("kvalid")
        xt = [k.sb("xt%d" % i, [128, D], F32) for i in range(2)]
        junk = k.sb("junk", [128, D], BF16)
        tmp = k.sb("tmpn", [128, D], F32)
        hb = [k.sb("hb%d" % i, [128, D], BF16) for i in range(2)]
        ms = [k.sb("ms%d" % i, [128, 4], F32) for i in range(2)]
        hT = k.sb("hT", [128, 16, 1024], BF16)
        hTb = [Buf("hT%d" % i) for i in range(8)]
        ptr = [k.ps("ptr%d" % i, [128, 1024], BF16) for i in range(2)]
        ropeg = k.sb("ropeg", [128, 2, 1024], F32)
        wst = [k.sb("wst%d" % i, [128, 16, 256], F32) for i in range(2)]
        wbf = [k.sb("wbf%d" % i, [128, 16, 256], BF16) for i in range(2)]
        pfm = [k.ps("pfm%d" % i, [128, 512]) for i in range(2)]
        psw = [k.ps("psw%d" % i, [128, 512]) for i in range(2)]
        ptm = [k.ps("ptm%d" % i, [128, 256]) for i in range(2)]
        qraw = [k.sb("qraw%d" % i, [128, 512], F32) for i in range(2)]
        t1 = [k.sb("t1_%d" % i, [128, 512], F32) for i in range(2)]
        t2 = [k.sb("t2_%d" % i, [128, 512], F32) for i in range(2)]
        stg = [k.sb("stg%d" % i, [128, 1024], BF16) for i in range(2)]
        stm = [k.sb("stm%d" % i, [128, 8, 256], BF16) for i in range(2)]
        stmf = k.sb("stmf", [128, 8, 36], F32)
        cnt = {"seg": 0, "fm": 0, "tm": 0, "st": 0, "sm": 0}

        kv_segs = []
        for i in range(3):
            kv_segs.append((768 + 256 * i, 256, "fm", True, "kTA", 256 * i))
        for i in range(3):
            kv_segs.append((1536 + 256 * i, 256, "tm", False, "vA", 256 * i))
        kv_segs.append((3072, 256, "fm", False, "cT", 0))
        kv_segs.append((3328, 128, "fm", False, "cT", 256))
        kv_segs.append((3456, 192, "fm", True, "kTS", 0))
        kv_segs.append((3648, 192, "tm", False, "vS", 0))
        kv_segs.append((3840, 192, "fm", True, "kTW", 0))
        kv_segs.append((4032, 192, "tm", False, "vW", 0))
        for i in range(2):
            kv_segs.append((4772 + 256 * i, 256, "fm", False, "kTC", 256 * i))
        for i in range(2):
            kv_segs.append((5284 + 256 * i, 256, "tm", False, "vC", 256 * i))
        q_segs = []
        for i in range(3):
            q_segs.append((256 * i, 256, "fm", True, "qTA", 256 * i))
        for i in range(3):
            q_segs.append((2304 + 256 * i, 256, "fm", True, "qTB", 256 * i))
        q_segs.append((4224, 36, "gate", False, "gates", 0))
        for i in range(2):
            q_segs.append((4260 + 256 * i, 256, "fm", False, "qTC", 256 * i))

        for g in range(5):
            own = (g == 4)
            k.dma("act", [(ropeg.t[:, 0, :], I["rope"][0, :, g * 1024:(g + 1) * 1024]),
                          (ropeg.t[:, 1, :], I["rope"][1, :, g * 1024:(g + 1) * 1024])], writes=[ropeg])
            for tt in range(8):
                x = xt[tt % 2]
                r0 = g * 1024 + tt * 128
                k.dma("sp", [(x.t[:], I["xin"][r0:r0 + 128, :])], writes=[x])
                h = hb[tt % 2]
                self.norm_tile(x, gamb, shb, h, ms[tt % 2], junk, tmp)
                for half in range(2):
                    p = ptr[half]
                    for kk in range(8):
                        kc = half * 8 + kk
                        k.op("pe", lambda e, p=p, kk=kk, kc=kc, h=h: e.transpose(
                            p.t[:, kk * 128:(kk + 1) * 128], h.t[:, kc * 128:(kc + 1) * 128], identb.t[:]),
                            reads=[h, identb], writes=[p])
                    dst = hT.t[:, half * 8:(half + 1) * 8, tt * 128:(tt + 1) * 128]
                    src = p.t[:].rearrange("p (a b) -> p a b", a=8)
                    eng = "act" if half == 0 else "dve"
                    if eng == "act":
                        k.op("act", lambda e, dst=dst, src=src: e.copy(dst, src), reads=[p], writes=[hTb[tt]])
                    else:
                        k.op("dve", lambda e, dst=dst, src=src: e.tensor_copy(dst, src), reads=[p], writes=[hTb[tt]])
            for (c0, w, kind, rope, dname, doff) in (q_segs if own else kv_segs):
                si = cnt["seg"] % 2
                cnt["seg"] += 1
                ws, wb = wst[si], wbf[si]
                src = I["w_in"][:, c0:c0 + w].rearrange("(k p) n -> p k n", p=128)
                k.dma("sp", [(ws.t[:, 4 * i:4 * i + 4, 0:w], src[:, 4 * i:4 * i + 4, :]) for i in range(4)], writes=[ws])
                if cnt["seg"] % 2 == 0:
                    k.op("dve", lambda e, ws=ws, wb=wb, w=w: e.tensor_copy(wb.t[:, :, 0:w], ws.t[:, :, 0:w]),
                         reads=[ws], writes=[wb])
                else:
                    k.op("act", lambda e, ws=ws, wb=wb, w=w: e.copy(wb.t[:, :, 0:w], ws.t[:, :, 0:w]),
                         reads=[ws], writes=[wb])
                dst = S[dname]
                if kind == "fm":
                    u0 = 0
                    while u0 < w:
                        uw = min(128, w - u0)
                        sg = stg[cnt["st"] % 2]
                        cnt["st"] += 1
                        for half in range(2):
                            p = pfm[cnt["fm"] % 2]
                            fi = cnt["fm"] % 2
                            cnt["fm"] += 1
                            for kk in range(16):
                                k.op("pe", lambda e, p=p, kk=kk, wb=wb, u0=u0, uw=uw, half=half: e.matmul(
                                    p.t[0:uw, :], wb.t[:, kk, u0:u0 + uw], hT.t[:, kk, half * 512:(half + 1) * 512],
                                    start=(kk == 0), stop=(kk == 15)),
                                    reads=[wb] + hTb[4 * half:4 * half + 4], writes=[p])
                            so = sg.t[0:uw, half * 512:(half + 1) * 512]
                            if not rope:
                                k.op("act", lambda e, so=so, p=p, uw=uw: e.copy(so, p.t[0:uw, :]), reads=[p], writes=[sg])
                            else:
                                qr, pw_, a1, a2 = qraw[fi], psw[fi], t1[fi], t2[fi]
                                k.op("act", lambda e, qr=qr, p=p, uw=uw: e.copy(qr.t[0:uw, :], p.t[0:uw, :]),
                                     reads=[p], writes=[qr])
                                k.op("pe", lambda e, pw_=pw_, qr=qr, uw=uw: e.matmul(
                                    pw_.t[0:uw, :], pswap.t[0:uw, 0:uw], qr.t[0:uw, :], start=True, stop=True),
                                    reads=[pswap, qr], writes=[pw_])
                                cs = ropeg.t[0:uw, 0, half * 512:(half + 1) * 512]
                                sn = ropeg.t[0:uw, 1, half * 512:(half + 1) * 512]
                                k.op("pool", lambda e, a1=a1, qr=qr, cs=cs, uw=uw: e.tensor_tensor(
                                    a1.t[0:uw, :], qr.t[0:uw, :], cs, ALU.mult), reads=[qr, ropeg], writes=[a1])
                                k.op("dve", lambda e, a2=a2, pw_=pw_, sn=sn, uw=uw: e.tensor_tensor(
                                    a2.t[0:uw, :], pw_.t[0:uw, :], sn, ALU.mult), reads=[pw_, ropeg], writes=[a2])
                                k.op("pool", lambda e, so=so, a1=a1, a2=a2, uw=uw: e.tensor_tensor(
                                    so, a1.t[0:uw, :], a2.t[0:uw, :], ALU.add), reads=[a1, a2], writes=[sg])
                        tok0 = 0 if own else g * 1024
                        k.dma("act", [(dst[doff + u0:doff + u0 + uw, tok0:tok0 + 1024], sg.t[0:uw, :])],
                              reads=[sg], writes=[self.SB[dname]])
                        u0 += uw
                else:
                    sm = stm[cnt["sm"] % 2] if kind == "tm" else stmf
                    cnt["sm"] += 1
                    for tt in range(8):
                        p = ptm[cnt["tm"] % 2]
                        cnt["tm"] += 1
                        for kk in range(16):
                            k.op("pe", lambda e, p=p, kk=kk, wb=wb, w=w, tt=tt: e.matmul(
                                p.t[:, 0:w], hT.t[:, kk, tt * 128:(tt + 1) * 128], wb.t[:, kk, 0:w],
                                start=(kk == 0), stop=(kk == 15)), reads=[wb, hTb[tt]], writes=[p])
                        if kind == "tm":
                            gt = g * 8 + tt
                            k.op("dve", lambda e, sm=sm, p=p, w=w, tt=tt, gt=gt: e.tensor_scalar(
                                sm.t[:, tt, 0:w], p.t[:, 0:w], kval.t[:, gt:gt + 1], None, op0=ALU.mult),
                                reads=[p, kval], writes=[sm])
                        else:
                            k.op("act", lambda e, sm=sm, p=p, w=w, tt=tt: e.activation(
                                sm.t[:, tt, 0:w], p.t[:, 0:w], AF.Sigmoid), reads=[p], writes=[sm])
                    tok0 = 0 if own else g * 1024
                    dv = dst[tok0:tok0 + 1024, doff:doff + w].rearrange("(t p) w -> p t w", p=128)
                    k.dma("act", [(dv, sm.t[:, :, 0:w])], reads=[sm], writes=[self.SB[dname]])

    def load_head(self, slot, kname, vname, qname, h, vcols=65, need_ones=True):
        k, S = self.k, self.S
        kT, va, qT = slot
        k.dma("sp", [(kT.t[:, i * 1024:(i + 1) * 1024], S[kname][h * 64:(h + 1) * 64, i * 1024:(i + 1) * 1024])
                     for i in range(4)], reads=[self.SB[kname]], writes=[kT])
        vsrc = S[vname][:, h * 64:(h + 1) * 64].rearrange("(t p) c -> p t c", p=128)
        k.dma("sp", [(va.t[:, i * 8:(i + 1) * 8, 0:64], vsrc[:, i * 8:(i + 1) * 8, :]) for i in range(4)],
              reads=[self.SB[vname]], writes=[va])
        if need_ones:
            k.op("pool", lambda e: e.tensor_copy(va.t[:, :, 64], self.kval.t[:, :]), reads=[self.kval, va], writes=[va])
        k.dma("sp", [(qT.t[:], S[qname][h * 64:(h + 1) * 64, :])], reads=[self.SB[qname]], writes=[qT])

    def head_slots(self, n=2):
        k = self.k
        return [(k.sb("kT%d" % i, [64, TL], BF16), k.sb("va%d" % i, [128, NT, 65], BF16),
                 k.sb("qT%d" % i, [64, NQ], BF16)) for i in range(n)]

    def norm_store(self, ost, nh, hg0):
        k, S = self.k, self.S
        W = nh * 64
        mixb = k.sb("mixb", [128, W], F32)
        k.dma("sp", [(mixb.t[:], self.I["mix_norm"][hg0 * 64:hg0 * 64 + W].partition_broadcast(128))], writes=[mixb])
        sq = k.sb("nsq", [128, W], F32)
        ss = k.sb("nss", [128, 3, nh], F32)
        on = [k.sb("non%d" % i, [128, W], BF16) for i in range(2)]
        for qt in range(8):
            o = ost.t[:, qt, :]
            k.op("dve", lambda e, o=o: e.tensor_tensor(sq.t[:], o, o, ALU.mult), reads=[ost], writes=[sq])
            k.op("dve", lambda e: e.tensor_reduce(ss.t[:, 0, :], sq.t[:].rearrange("p (h d) -> p h d", d=64),
                                                  AX.X, ALU.add), reads=[sq], writes=[ss])
            k.op("act", lambda e: e.activation(ss.t[:, 1, :], ss.t[:, 0, :], AF.Sqrt, bias=EPS, scale=1.0 / 64),
                 reads=[ss], writes=[ss])
            k.op("dve", lambda e: e.reciprocal(ss.t[:, 2, :], ss.t[:, 1, :]), reads=[ss], writes=[ss])
            rb = ss.t[:, 2, :].unsqueeze(2).to_broadcast([128, nh, 64])
            k.op("dve", lambda e, o=o, rb=rb: e.tensor_tensor(sq.t[:].rearrange("p (h d) -> p h d", d=64),
                                                             o.rearrange("p (h d) -> p h d", d=64), rb, ALU.mult),
                 reads=[ost, ss], writes=[sq])
            ob = on[qt % 2]
            k.op("pool", lambda e, ob=ob: e.tensor_tensor(ob.t[:], sq.t[:], mixb.t[:], ALU.mult),
                 reads=[sq, mixb], writes=[ob])
            k.dma("act", [(S["otok"][qt * 128:(qt + 1) * 128, hg0 * 64:hg0 * 64 + W], ob.t[:])], reads=[ob],
                  writes=[self.SB["otok"]])

    def softmax_branch(self, kT, va, qT, qg, jlist, maskfn, acc, st, E, Em, cnt, vw=65, qap=None):
        k = self.k
        if qap is None:
            qap = qT.t[:, qg * 512:(qg + 1) * 512]
        base = cnt[0]
        cnt[0] += len(jlist)

        def emit_st(ii):
            sx = st[(base + ii) % len(st)]
            jx = jlist[ii]
            k.op("pe", lambda e: e.matmul(sx.t[:], kT.t[:, jx * 128:(jx + 1) * 128], qap, start=True, stop=True),
                 reads=[kT, qT], writes=[sx])

        emit_st(0)
        for ji, j in enumerate(jlist):
            s_ = st[(base + ji) % len(st)]
            e_ = E[(base + ji) % len(E)]
            m_ = Em[(base + ji) % len(Em)]
            if ji + 1 < len(jlist):
                emit_st(ji + 1)
            k.op("act", lambda e, s_=s_, e_=e_: e.activation(e_.t[:], s_.t[:], AF.Exp, scale=SCALE),
                 reads=[s_], writes=[e_])
            mk = maskfn(j)
            if mk is not None:
                mt, map_ = mk
                k.op("dve", lambda e, m_=m_, e_=e_, map_=map_: e.tensor_tensor(m_.t[:], e_.t[:], map_, ALU.mult),
                     reads=[e_, mt], writes=[m_])
                src = m_
            else:
                src = e_
            for qt in range(4):
                k.op("pe", lambda e, src=src, qt=qt, j=j, ji=ji: e.matmul(
                    acc.t[:, qt, 0:vw], src.t[:, qt * 128:(qt + 1) * 128], va.t[:, j, 0:vw],
                    start=(ji == 0 and qt == 0), stop=(ji == len(jlist) - 1), skip_group_check=True),
                     reads=[src, va], writes=[acc])

    def phase_att_a(self):
        k = self.k
        self.kval = self.load_const("kvalid")
        maskA = self.load_const("maskA", rearr="a p n -> p a n")
        slots = self.head_slots()
        st = [k.ps("st%d" % i, [128, 512]) for i in range(4)]
        acc = [k.ps("acc%d" % i, [128, 4, 128]) for i in range(2)]
        E = [k.sb("E%d" % i, [128, 512], BF16) for i in range(4)]
        Em = [k.sb("Em%d" % i, [128, 512], BF16) for i in range(4)]
        ost = k.sb("ostA", [128, 8, 768], F32)
        rec = k.sb("rec", [128, 4], F32)
        cnt = [0]
        self.load_head(slots[0], "kTA", "vA", "qTA", 0)
        for h in range(12):
            if h + 1 < 12:
                self.load_head(slots[(h + 1) % 2], "kTA", "vA", "qTA", h + 1)
            kT, va, qT = slots[h % 2]
            for qg in range(2):
                i0 = 24 + 4 * qg
                a = acc[(2 * h + qg) % 2]
                self.softmax_branch(kT, va, qT, qg, list(range(i0 - 16, i0 + 4)),
                                    lambda j: (maskA, maskA.t[:, i0 - j + 3, :]), a, st, E, Em, cnt)
                k.op("dve", lambda e, a=a: e.reciprocal(rec.t[:], a.t[:, :, 64]), reads=[a], writes=[rec])
                for qt in range(4):
                    k.op("dve", lambda e, a=a, qt=qt, h=h, qg=qg: e.tensor_scalar(
                        ost.t[:, qg * 4 + qt, h * 64:(h + 1) * 64], a.t[:, qt, 0:64], rec.t[:, qt:qt + 1], None,
                        op0=ALU.mult), reads=[a, rec], writes=[ost])
        self.norm_store(ost, 12, 0)

    def phase_att_c(self):
        k = self.k
        self.kval = self.load_const("kvalid")
        u2 = self.load_const("u2")
        onesf = self.load_const("onesf")
        mSF = self.load_const("maskStrictF", rearr="a p n -> p a n")
        mSB = self.load_const("maskStrictB", rearr="a p n -> p a n")
        slots = self.head_slots()
        zp = [k.ps("zp%d" % i, [128, 512]) for i in range(3)]
        sp = [k.ps("sp%d" % i, [128, 512]) for i in range(3)]
        acc = [k.ps("accc%d" % i, [128, 4, 128]) for i in range(2)]
        e1 = [k.sb("e1_%d" % i, [128, 512], F32) for i in range(3)]
        Lp = [k.sb("Lp%d" % i, [128, 512], F32) for i in range(3)]
        Lm = [k.sb("Lm%d" % i, [128, 512], F32) for i in range(3)]
        t1 = [k.sb("ct1_%d" % i, [128, 512], F32) for i in range(3)]
        arg = [k.sb("arg%d" % i, [128, 512], F32) for i in range(3)]
        av = [k.sb("av%d" % i, [128, 512], BF16) for i in range(3)]
        am = [k.sb("am%d" % i, [128, 512], BF16) for i in range(3)]
        Lacc = k.sb("Lacc", [128, 512], F32)
        ost = k.sb("ostC", [128, 8, 512], F32)
        c = 0
        self.load_head(slots[0], "kTC", "vC", "qTC", 0, need_ones=False)
        for h in range(8):
            if h + 1 < 8:
                self.load_head(slots[(h + 1) % 2], "kTC", "vC", "qTC", h + 1, need_ones=False)
            kT, va, qT = slots[h % 2]
            for qg in range(2):
                i0 = 24 + 4 * qg
                a_ = acc[(2 * h + qg) % 2]
                jl = list(range(i0 + 3, -1, -1))
                k.op("pool", lambda e: e.memset(Lacc.t[:], 0.0), writes=[Lacc])
                def emit_z(ii, cbase):
                    zx = zp[(cbase + ii) % 3]
                    jx = jl[ii]
                    k.op("pe", lambda e: e.matmul(zx.t[:], kT.t[:, jx * 128:(jx + 1) * 128],
                                                  qT.t[:, qg * 512:(qg + 1) * 512], start=True, stop=True),
                         reads=[kT, qT], writes=[zx])

                cbase = c
                emit_z(0, cbase)
                for ji, j in enumerate(jl):
                    b = c % 3
                    c += 1
                    z, s_ = zp[b], sp[b]
                    if ji + 1 < len(jl):
                        emit_z(ji + 1, cbase)
                    k.op("act", lambda e, z=z, b=b: e.activation(e1[b].t[:], z.t[:], AF.Exp, scale=SCALE),
                         reads=[z], writes=[e1[b]])
                    k.op("act", lambda e, b=b: e.activation(Lp[b].t[:], e1[b].t[:], AF.Ln, bias=1.0, scale=1.0),
                         reads=[e1[b]], writes=[Lp[b]])
                    if j >= i0:
                        k.op("dve", lambda e, b=b, j=j: e.tensor_tensor(Lm[b].t[:], Lp[b].t[:], mSF.t[:, j - i0, :], ALU.mult),
                             reads=[Lp[b], mSF], writes=[Lm[b]])
                    else:
                        k.op("dve", lambda e, b=b, j=j: e.tensor_scalar(Lm[b].t[:], Lp[b].t[:], self.kval.t[:, j:j + 1], None,
                                                                        op0=ALU.mult), reads=[Lp[b], self.kval], writes=[Lm[b]])
                    k.op("pe", lambda e, s_=s_, b=b, ji=ji: e.matmul(s_.t[:], u2.t[:], Lm[b].t[:], start=True, stop=(ji == 0)),
                         reads=[u2, Lm[b]], writes=[s_])
                    if ji > 0:
                        k.op("pe", lambda e, s_=s_: e.matmul(s_.t[:], onesf.t[:], Lacc.t[:], start=False, stop=True),
                             reads=[onesf, Lacc], writes=[s_])
                    k.op("dve", lambda e, z=z, b=b: e.scalar_tensor_tensor(t1[b].t[:], z.t[:], SCALE, Lp[b].t[:],
                                                                           ALU.mult, ALU.subtract),
                         reads=[z, Lp[b]], writes=[t1[b]])
                    k.op("dve", lambda e, s_=s_, b=b: e.tensor_tensor(arg[b].t[:], t1[b].t[:], s_.t[:], ALU.subtract),
                         reads=[t1[b], s_], writes=[arg[b]])
                    k.op("act", lambda e, b=b: e.activation(av[b].t[:], arg[b].t[:], AF.Exp), reads=[arg[b]], writes=[av[b]])
                    src = av[b]
                    if j >= i0:
                        k.op("pool", lambda e, b=b, j=j: e.tensor_tensor(am[b].t[:], av[b].t[:], mSB.t[:, j - i0, :], ALU.mult),
                             reads=[av[b], mSB], writes=[am[b]])
                        src = am[b]
                    k.op("dve", lambda e, b=b: e.tensor_tensor(Lacc.t[:], Lacc.t[:], Lm[b].t[:], ALU.add),
                         reads=[Lacc, Lm[b]], writes=[Lacc])
                    for qt in range(4):
                        k.op("pe", lambda e, src=src, qt=qt, j=j, ji=ji: e.matmul(
                            a_.t[:, qt, 0:64], src.t[:, qt * 128:(qt + 1) * 128], va.t[:, j, 0:64],
                            start=(ji == 0 and qt == 0), stop=(ji == len(jl) - 1), skip_group_check=True),
                            reads=[src, va], writes=[a_])
                k.op("act", lambda e, a_=a_, h=h, qg=qg: e.copy(ost.t[:, qg * 4:(qg + 1) * 4, h * 64:(h + 1) * 64],
                                                             a_.t[:, :, 0:64]), reads=[a_], writes=[ost])
        self.norm_store(ost, 8, 24)

    def phase_att_b(self):
        k, I, S = self.k, self.I, self.S
        self.kval = self.load_const("kvalid")
        cval = self.load_const("cvalid")
        ovl = self.load_const("ovl")
        identb = self.load_const("identb")
        mCmp = self.load_const("maskCmp", rearr="a b p n -> p (a b) n")
        mCaus = self.load_const("maskCaus", rearr="a p n -> p a n")
        mW = self.load_const("maskW", rearr="a p n -> p a n")
        expand = self.load_const("expand")
        impb_t = k.sb("impbias", [128, 8, 64], F32)
        k.dma("sp", [(impb_t.t[:], I["impbias"].rearrange("(t p) m -> p t m", p=128))], writes=[impb_t])
        gates = k.sb("gates", [128, 8, 36], F32)
        k.dma("sp", [(gates.t[:], S["gates"].rearrange("(t p) c -> p t c", p=128))], reads=[self.SB["gates"]],
              writes=[gates])
        kcT = k.sb("kcT", [64, 3, 256], BF16)
        rhsC = k.sb("rhsC", [128, 3, 2, 129], BF16)
        k.op("pool", lambda e: e.memset(kcT.t[:], 0.0), writes=[kcT])
        for g in range(3):
            k.op("pool", lambda e, g=g: e.tensor_copy(rhsC.t[:, g, :, 64], cval.t[:, :]), reads=[cval], writes=[rhsC])
            k.op("pool", lambda e, g=g: e.tensor_copy(rhsC.t[:, g, :, 65:129], ovl.t[:, :, :]), reads=[ovl], writes=[rhsC])
        with k.scope():
            pswap = self.load_const("pswap")
            ropec = k.sb("ropec", [64, 2, 256], F32)
            k.dma("sp", [(ropec.t[:, 0, :], I["ropec"][0]), (ropec.t[:, 1, :], I["ropec"][1])], writes=[ropec])
            w1f = k.sb("w1f", [64, 32, 128], F32)
            w1b = k.sb("w1b", [64, 32, 128], BF16)
            w2f = k.sb("w2f", [128, 64], F32)
            w2b = k.sb("w2b", [128, 64], BF16)
            pef = k.sb("pef", [64, 32], F32)
            peb = k.sb("peb", [64, 32], BF16)
            cb = k.sb("cb", [128, 1], F32)
            xk = [k.sb("xk%d" % i, [64, TL], BF16) for i in range(2)]
            u = k.sb("cu", [128, 255], F32)
            uu = k.sb("cuu", [128, 255], F32)
            sg = k.sb("csg", [128, 255], F32)
            hid = k.sb("hid", [128, 256], BF16)
            kraw = k.sb("kraw", [64, 255], F32)
            ka1 = k.sb("ka1", [64, 255], F32)
            ka2 = k.sb("ka2", [64, 255], F32)
            pb = k.ps("pcb", [128, 1])
            hp = k.ps("hp", [128, 255])
            kp = k.ps("kp", [64, 255])
            kw = k.ps("kw", [64, 255])
            vp = [k.ps("vp%d" % i, [128, 64]) for i in range(2)]
            k.op("pool", lambda e: e.memset(hid.t[:], 0.0), writes=[hid])
            ci = 0
            for kv in range(2):
                k.dma("sp", [(w1f.t[:, 8 * i:8 * i + 8, :],
                              I["cmp_w1"][kv].rearrange("(l d) h -> d l h", d=64)[:, 8 * i:8 * i + 8, :]) for i in range(4)],
                      writes=[w1f])
                k.op("pool", lambda e: e.tensor_copy(w1b.t[:], w1f.t[:]), reads=[w1f], writes=[w1b])
                k.dma("sp", [(w2f.t[:], I["cmp_w2"][kv])], writes=[w2f])
                k.op("pool", lambda e: e.tensor_copy(w2b.t[:], w2f.t[:]), reads=[w2f], writes=[w2b])
                k.dma("sp", [(pef.t[:], I["cmp_pe"][kv].rearrange("l d -> d l"))], writes=[pef],
                      allow_slow_non_contiguous=True)
                k.op("pool", lambda e: e.tensor_copy(peb.t[:], pef.t[:]), reads=[pef], writes=[peb])
                for l in range(32):
                    k.op("pe", lambda e, l=l: e.matmul(pb.t[:], w1b.t[:, l, :], peb.t[:, l:l + 1],
                                                       start=(l == 0), stop=(l == 31)), reads=[w1b, peb], writes=[pb])
                k.op("act", lambda e: e.copy(cb.t[:], pb.t[:]), reads=[pb], writes=[cb])
                for g in range(3):
                    x_ = xk[ci % 2]
                    ci += 1
                    r0 = kv * 192 + g * 64
                    k.dma("sp", [(x_.t[:, i * 1024:(i + 1) * 1024], S["cT"][r0:r0 + 64, i * 1024:(i + 1) * 1024])
                                 for i in range(4)], reads=[self.SB["cT"]], writes=[x_])
                    for l in range(32):
                        k.op("pe", lambda e, l=l, x_=x_: e.matmul(hp.t[:], w1b.t[:, l, :], x_.t[:, l:l + 4065:16],
                                                                  start=(l == 0), stop=(l == 31)),
                             reads=[w1b, x_], writes=[hp])
                    k.op("act", lambda e: e.activation(u.t[:], hp.t[:], AF.Identity, bias=cb.t[:, 0:1], scale=1.0),
                         reads=[hp, cb], writes=[u])
                    k.op("dve", lambda e: e.tensor_tensor(uu.t[:], u.t[:], u.t[:], ALU.mult), reads=[u], writes=[uu])
                    k.op("dve", lambda e: e.tensor_scalar(uu.t[:], uu.t[:], 0.044715, 1.0, op0=ALU.mult, op1=ALU.add),
                         reads=[uu], writes=[uu])
                    k.op("dve", lambda e: e.tensor_tensor(uu.t[:], uu.t[:], u.t[:], ALU.mult), reads=[uu, u], writes=[uu])
                    k.op("act", lambda e: e.activation(sg.t[:], uu.t[:], AF.Sigmoid, scale=1.5957691216057308),
                         reads=[uu], writes=[sg])
                    k.op("dve", lambda e: e.tensor_tensor(hid.t[:, 0:255], u.t[:], sg.t[:], ALU.mult),
                         reads=[u, sg], writes=[hid])
                    if kv == 0:
                        k.op("pe", lambda e: e.matmul(kp.t[:], w2b.t[:], hid.t[:, 0:255], start=True, stop=True),
                             reads=[w2b, hid], writes=[kp])
                        k.op("act", lambda e: e.copy(kraw.t[:], kp.t[:]), reads=[kp], writes=[kraw])
                        k.op("pe", lambda e: e.matmul(kw.t[:], pswap.t[0:64, 0:64], kraw.t[:], start=True, stop=True),
                             reads=[pswap, kraw], writes=[kw])
                        k.op("pool", lambda e: e.tensor_tensor(ka1.t[:], kraw.t[:], ropec.t[:, 0, 0:255], ALU.mult),
                             reads=[kraw, ropec], writes=[ka1])
                        k.op("dve", lambda e: e.tensor_tensor(ka2.t[:], kw.t[:], ropec.t[:, 1, 0:255], ALU.mult),
                             reads=[kw, ropec], writes=[ka2])
                        k.op("pool", lambda e, g=g: e.tensor_tensor(kcT.t[:, g, 0:255], ka1.t[:], ka2.t[:], ALU.add),
                             reads=[ka1, ka2], writes=[kcT])
                    else:
                        for nt in range(2):
                            v_ = vp[nt]
                            k.op("pe", lambda e, nt=nt, v_=v_: e.matmul(v_.t[:], hid.t[:, nt * 128:(nt + 1) * 128], w2b.t[:],
                                                                        start=True, stop=True), reads=[hid, w2b], writes=[v_])
                            k.op("dve", lambda e, nt=nt, g=g, v_=v_: e.tensor_scalar(
                                rhsC.t[:, g, nt, 0:64], v_.t[:], cval.t[:, nt:nt + 1], None, op0=ALU.mult),
                                reads=[v_, cval], writes=[rhsC])
        kS = k.sb("kS", [64, TL], BF16)
        kW = k.sb("kW", [64, TL], BF16)
        vaS = k.sb("vaS", [128, NT, 65], BF16)
        vaW = k.sb("vaW", [128, NT, 65], BF16)
        qT4 = k.sb("qT4", [64, 4, 512], BF16)
        st = [k.ps("st%d" % i, [128, 512]) for i in range(2)]
        accC = k.ps("accC", [128, 4, 256])
        accS = k.ps("accS", [128, 4, 128])
        accW = k.ps("accW", [128, 4, 128])
        mp = k.ps("mp", [128, 512])
        selTp = k.ps("selTp", [64, 512], BF16)
        E = [k.sb("E%d" % i, [128, 512], BF16) for i in range(2)]
        Em = [k.sb("Em%d" % i, [128, 512], BF16) for i in range(2)]
        maskall = k.sb("maskall", [128, NT, 512], BF16)
        maskb = [Buf("maskall%d" % j) for j in range(NT)]
        ocmp = k.sb("ocmp", [128, 4, 4, 64], F32)
        impg = k.sb("impg", [128, 4, 64], F32)
        recC = k.sb("recC", [128, 4], F32)
        recS = k.sb("recS", [128, 2, 4], F32)
        wsw = k.sb("wsw", [128, 2, 4], F32)
        impb = k.sb("impb", [128, 64], F32)
        imt = k.sb("imt", [128, 64], F32)
        m8 = k.sb("m8", [128, 2, 8], F32)
        selb = k.sb("selb", [128, 64], BF16)
        selT = k.sb("selT", [64, 512], BF16)
        otmp = k.sb("otmp", [128, 64], F32)
        ost = k.sb("ostB", [128, 8, 768], F32)
        cnt = [0]
        for g in range(3):
            r0 = g * 64
            k.dma("sp", [(kS.t[:, i * 1024:(i + 1) * 1024], S["kTS"][r0:r0 + 64, i * 1024:(i + 1) * 1024]) for i in range(4)],
                  reads=[self.SB["kTS"]], writes=[kS])
            k.dma("sp", [(kW.t[:, i * 1024:(i + 1) * 1024], S["kTW"][r0:r0 + 64, i * 1024:(i + 1) * 1024]) for i in range(4)],
                  reads=[self.SB["kTW"]], writes=[kW])
            for (va, nm) in ((vaS, "vS"), (vaW, "vW")):
                vsrc = S[nm][:, r0:r0 + 64].rearrange("(t p) c -> p t c", p=128)
                k.dma("sp", [(va.t[:, i * 8:(i + 1) * 8, 0:64], vsrc[:, i * 8:(i + 1) * 8, :]) for i in range(4)],
                      reads=[self.SB[nm]], writes=[va])
                k.op("pool", lambda e, va=va: e.tensor_copy(va.t[:, :, 64], self.kval.t[:, :]), reads=[self.kval, va],
                     writes=[va])
            for qg in range(2):
                i0 = 24 + 4 * qg
                k.dma("sp", [(qT4.t[:, r, :], S["qTB"][(4 * g + r) * 64:(4 * g + r + 1) * 64, qg * 512:(qg + 1) * 512])
                             for r in range(4)], reads=[self.SB["qTB"]], writes=[qT4])
                for r in range(4):
                    for nt in range(2):
                        s_ = st[cnt[0] % 2]
                        e_ = E[cnt[0] % 2]
                        m_ = Em[cnt[0] % 2]
                        cnt[0] += 1
                        k.op("pe", lambda e, s_=s_, nt=nt, r=r: e.matmul(s_.t[:], kcT.t[:, g, nt * 128:(nt + 1) * 128],
                                                                         qT4.t[:, r, :], start=True, stop=True),
                             reads=[kcT, qT4], writes=[s_])
                        k.op("act", lambda e, s_=s_, e_=e_: e.activation(e_.t[:], s_.t[:], AF.Exp, scale=SCALE),
                             reads=[s_], writes=[e_])
                        k.op("dve", lambda e, m_=m_, e_=e_, nt=nt: e.tensor_tensor(m_.t[:], e_.t[:], mCmp.t[:, qg * 2 + nt, :],
                                                                                  ALU.mult), reads=[e_, mCmp], writes=[m_])
                        for qt in range(4):
                            k.op("pe", lambda e, m_=m_, qt=qt, nt=nt: e.matmul(
                                accC.t[:, qt, 0:129], m_.t[:, qt * 128:(qt + 1) * 128], rhsC.t[:, g, nt, :],
                                start=(nt == 0 and qt in (0, 2)), stop=(nt == 1), skip_group_check=True),
                                reads=[m_, rhsC], writes=[accC])
                    k.op("dve", lambda e: e.tensor_scalar(recC.t[:], accC.t[:, :, 64], 1e-30, None, op0=ALU.max),
                         reads=[accC], writes=[recC])
                    k.op("dve", lambda e: e.reciprocal(recC.t[:], recC.t[:]), reads=[recC], writes=[recC])
                    for qt in range(4):
                        k.op("dve", lambda e, r=r, qt=qt: e.tensor_scalar(ocmp.t[:, r, qt, :], accC.t[:, qt, 0:64],
                                                                          recC.t[:, qt:qt + 1], None, op0=ALU.mult),
                             reads=[accC, recC], writes=[ocmp])
                        if r == 0:
                            k.op("dve", lambda e, qt=qt: e.tensor_scalar(impg.t[:, qt, :], accC.t[:, qt, 65:129],
                                                                         recC.t[:, qt:qt + 1], None, op0=ALU.mult),
                                 reads=[accC, recC], writes=[impg])
                        else:
                            k.op("dve", lambda e, qt=qt: e.scalar_tensor_tensor(
                                impg.t[:, qt, :], accC.t[:, qt, 65:129], recC.t[:, qt:qt + 1], impg.t[:, qt, :],
                                ALU.mult, ALU.add), reads=[accC, recC, impg], writes=[impg])
                for qt in range(4):
                    k.op("dve", lambda e, qt=qt: e.tensor_tensor(impb.t[:], impg.t[:, qt, :], impb_t.t[:, qg * 4 + qt, :],
                                                                 ALU.add), reads=[impg, impb_t], writes=[impb])
                    k.op("dve", lambda e: e.max(m8.t[:, 0, :], impb.t[:]), reads=[impb], writes=[m8])
                    k.op("dve", lambda e: e.match_replace(imt.t[:], m8.t[:, 0, :], impb.t[:], -1e30),
                         reads=[m8, impb], writes=[imt])
                    k.op("dve", lambda e: e.max(m8.t[:, 1, :], imt.t[:]), reads=[imt], writes=[m8])
                    k.op("dve", lambda e: e.tensor_scalar(selb.t[:], impb.t[:], m8.t[:, 1, 7:8], None, op0=ALU.is_ge),
                         reads=[impb, m8], writes=[selb])
                    k.op("pe", lambda e, qt=qt: e.transpose(selTp.t[:, qt * 128:(qt + 1) * 128], selb.t[:], identb.t[:]),
                         reads=[selb, identb], writes=[selTp])
                k.op("act", lambda e: e.copy(selT.t[:], selTp.t[:]), reads=[selTp], writes=[selT])
                for j in range(i0 + 4):
                    k.op("pe", lambda e, j=j: e.matmul(mp.t[:], expand.t[:, j * 128:(j + 1) * 128], selT.t[:],
                                                       start=True, stop=True), reads=[expand, selT], writes=[mp])
                    if j >= i0:
                        k.op("dve", lambda e, j=j: e.tensor_tensor(maskall.t[:, j, :], mp.t[:], mCaus.t[:, j - i0, :], ALU.mult),
                             reads=[mp, mCaus], writes=[maskb[j]])
                    else:
                        k.op("act", lambda e, j=j: e.copy(maskall.t[:, j, :], mp.t[:]), reads=[mp], writes=[maskb[j]])
                for r in range(4):
                    h = 4 * g + r
                    self.softmax_branch(kS, vaS, qT4, qg, list(range(i0 + 4)),
                                        lambda j: (maskb[j], maskall.t[:, j, :]), accS, st, E, Em, cnt, qap=qT4.t[:, r, :])
                    self.softmax_branch(kW, vaW, qT4, qg, list(range(i0 - 4, i0 + 4)),
                                        lambda j: (mW, mW.t[:, j - i0 + 4, :]), accW, st, E, Em, cnt, qap=qT4.t[:, r, :])
                    k.op("dve", lambda e: e.tensor_scalar(recS.t[:, 0, :], accS.t[:, :, 64], 1e-30, None, op0=ALU.max),
                         reads=[accS], writes=[recS])
                    k.op("dve", lambda e: e.tensor_scalar(recS.t[:, 1, :], accW.t[:, :, 64], 1e-30, None, op0=ALU.max),
                         reads=[accW], writes=[recS])
                    k.op("dve", lambda e: e.reciprocal(recS.t[:], recS.t[:]), reads=[recS], writes=[recS])
                    for br in (1, 2):
                        k.op("dve", lambda e, br=br, h=h: e.tensor_tensor(
                            wsw.t[:, br - 1, :], recS.t[:, br - 1, :], gates.t[:, qg * 4:(qg + 1) * 4, h * 3 + br], ALU.mult),
                            reads=[recS, gates], writes=[wsw])
                    for qt in range(4):
                        qq = qg * 4 + qt
                        k.op("dve", lambda e, r=r, qt=qt, qq=qq, h=h: e.tensor_scalar(
                            otmp.t[:], ocmp.t[:, r, qt, :], gates.t[:, qq, h * 3:h * 3 + 1], None, op0=ALU.mult),
                            reads=[ocmp, gates], writes=[otmp])
                        k.op("dve", lambda e, qt=qt: e.scalar_tensor_tensor(
                            otmp.t[:], accS.t[:, qt, 0:64], wsw.t[:, 0, qt:qt + 1], otmp.t[:], ALU.mult, ALU.add),
                            reads=[accS, wsw, otmp], writes=[otmp])
                        k.op("dve", lambda e, qt=qt, qq=qq, h=h: e.scalar_tensor_tensor(
                            ost.t[:, qq, h * 64:(h + 1) * 64], accW.t[:, qt, 0:64], wsw.t[:, 1, qt:qt + 1], otmp.t[:],
                            ALU.mult, ALU.add), reads=[accW, wsw, otmp], writes=[ost])
        self.norm_store(ost, 12, 12)

    def phase_oproj(self):
        k, I, S = self.k, self.I, self.S
        identb = self.load_const("identb")
        g1b = self.bvec(2, "g1b")
        woutb = k.sb("woutb", [128, 16, D], BF16)
        wob = [Buf("wob%d" % i) for i in range(8)]
        wst = [k.sb("wost%d" % i, [128, 16, 256], F32) for i in range(2)]
        for c in range(8):
            ws = wst[c % 2]
            src = I["w_out"][:, c * 256:(c + 1) * 256].rearrange("(k p) n -> p k n", p=128)
            k.dma("sp", [(ws.t[:, 4 * i:4 * i + 4, :], src[:, 4 * i:4 * i + 4, :]) for i in range(4)], writes=[ws])
            eng = "pool" if c % 2 == 0 else "act"
            if eng == "pool":
                k.op("dve", lambda e, ws=ws, c=c: e.tensor_copy(woutb.t[:, :, c * 256:(c + 1) * 256], ws.t[:]),
                     reads=[ws], writes=[wob[c]])
            else:
                k.op("act", lambda e, ws=ws, c=c: e.copy(woutb.t[:, :, c * 256:(c + 1) * 256], ws.t[:]),
                     reads=[ws], writes=[wob[c]])
        ot = [k.sb("ot%d" % i, [128, D], BF16) for i in range(2)]
        xq = [k.sb("xq%d" % i, [128, D], F32) for i in range(2)]
        x1 = [k.sb("x1_%d" % i, [128, D], F32) for i in range(2)]
        tmp = k.sb("otmp", [128, 512], F32)
        OT = [k.sb("OT%d" % i, [128, 16, 128], BF16) for i in range(2)]
        ptr = [k.ps("optr%d" % i, [128, 1024], BF16) for i in range(2)]
        po = [k.ps("po%d" % i, [128, 512]) for i in range(2)]
        n = 0
        for qt in range(8):
            o_, x_, x1_, OT_ = ot[qt % 2], xq[qt % 2], x1[qt % 2], OT[qt % 2]
            k.dma("sp", [(o_.t[:], S["otok"][qt * 128:(qt + 1) * 128, :])], reads=[self.SB["otok"]], writes=[o_])
            k.dma("sp", [(x_.t[:], I["xin"][TL + qt * 128:TL + (qt + 1) * 128, :])], writes=[x_])
            for half in range(2):
                p = ptr[half]
                for kk in range(8):
                    kc = half * 8 + kk
                    k.op("pe", lambda e, p=p, kk=kk, kc=kc, o_=o_: e.transpose(
                        p.t[:, kk * 128:(kk + 1) * 128], o_.t[:, kc * 128:(kc + 1) * 128], identb.t[:]),
                        reads=[o_, identb], writes=[p])
                dst = OT_.t[:, half * 8:(half + 1) * 8, :]
                src = p.t[:].rearrange("p (a b) -> p a b", a=8)
                if half == 0:
                    k.op("act", lambda e, dst=dst, src=src: e.copy(dst, src), reads=[p], writes=[OT_])
                else:
                    k.op("dve", lambda e, dst=dst, src=src: e.tensor_copy(dst, src), reads=[p], writes=[OT_])
            for c in range(4):
                p = po[n % 2]
                n += 1
                for kk in range(16):
                    k.op("pe", lambda e, p=p, kk=kk, c=c, OT_=OT_: e.matmul(
                        p.t[:], OT_.t[:, kk, :], woutb.t[:, kk, c * 512:(c + 1) * 512], start=(kk == 0), stop=(kk == 15)),
                        reads=[OT_, wob[2 * c], wob[2 * c + 1]], writes=[p])
                k.op("dve", lambda e, p=p, c=c: e.tensor_tensor(tmp.t[:], p.t[:], g1b.t[:, c * 512:(c + 1) * 512], ALU.mult),
                     reads=[p, g1b], writes=[tmp])
                k.op("pool", lambda e, c=c, x_=x_, x1_=x1_: e.tensor_tensor(
                    x1_.t[:, c * 512:(c + 1) * 512], tmp.t[:], x_.t[:, c * 512:(c + 1) * 512], ALU.add),
                    reads=[tmp, x_], writes=[x1_])
            k.dma("act", [(S["x1"][qt * 128:(qt + 1) * 128, :], x1_.t[:])], reads=[x1_], writes=[self.SB["x1"]])

    def phase_moe(self):
        k, I, S = self.k, self.I, self.S
        identb = self.load_const("identb")
        tri = self.load_const("tri")
        onesb = self.load_const("onesb")
        iota = self.load_const("iota")
        xacc = k.sb("xacc", [128, 8, D], F32)
        xab = [Buf("xacc%d" % i) for i in range(8)]
        maskf = k.sb("maskf", [128, 8, NE], F32)
        maskb = k.sb("maskb", [128, 8, NE], BF16)
        gate = k.sb("gate", [128, 8, NE], F32)
        pos = k.sb("pos", [128, 8, NE], F32)
        k.op("pool", lambda e: e.memset(xacc.t[:], 0.0), writes=xab)
        with k.scope():
            identf = self.load_const("identf")
            gam2b = self.bvec(3, "gam2b")
            sh2b = self.bvec(4, "sh2b", q="act")
            wr = k.sb("wr", [128, 16, NE], F32)
            k.dma("sp", [(wr.t[:], I["w_router"].rearrange("(k p) e -> p k e", p=128))], writes=[wr])
            brb = k.sb("brb", [128, NE], F32)
            k.dma("sp", [(brb.t[:], I["b_router"].partition_broadcast(128))], writes=[brb])
            xt = [k.sb("mxt%d" % i, [128, D], F32) for i in range(2)]
            junk = k.sb("mjunk", [128, D], BF16)
            tmp = k.sb("mtmp", [128, D], F32)
            h2f = [k.sb("h2f%d" % i, [128, D], F32) for i in range(2)]
            h2bf = [k.sb("h2bf%d" % i, [128, D], BF16) for i in range(2)]
            ms = [k.sb("mms%d" % i, [128, 4], F32) for i in range(2)]
            hTf = k.sb("hTf", [128, 16, 128], F32)
            ptrf = [k.ps("ptrf%d" % i, [128, 512]) for i in range(2)]
            plg = k.ps("plg", [128, NE])
            ppos = k.ps("ppos", [128, NE])
            lg = k.sb("lg", [128, NE], F32)
            m8 = k.sb("rm8", [128, 8], F32)
            sm = k.sb("rsm", [128, 4], F32)
            ex = k.sb("rex", [128, NE], F32)
            for qt in range(8):
                x_, h_ = xt[qt % 2], h2f[qt % 2]
                k.dma("sp", [(x_.t[:], S["x1"][qt * 128:(qt + 1) * 128, :])], reads=[self.SB["x1"]], writes=[x_])
                self.norm_tile(x_, gam2b, sh2b, h_, ms[qt % 2], junk, tmp)
                hb_ = h2bf[qt % 2]
                k.op("act", lambda e, h_=h_, hb_=hb_: e.copy(hb_.t[:], h_.t[:]), reads=[h_], writes=[hb_])
                k.dma("act", [(S["h2s"][:, :, qt, :].rearrange("k p f -> p k f"), hb_.t[:].rearrange("p (k f) -> p k f", f=128))],
                      reads=[hb_], writes=[self.SB["h2s"]])
                for rnd in range(4):
                    p = ptrf[rnd % 2]
                    for kk in range(4):
                        kc = rnd * 4 + kk
                        k.op("pe", lambda e, p=p, kk=kk, kc=kc, h_=h_: e.transpose(
                            p.t[:, kk * 128:(kk + 1) * 128], h_.t[:, kc * 128:(kc + 1) * 128], identf.t[:]),
                            reads=[h_, identf], writes=[p])
                    dst = hTf.t[:, rnd * 4:(rnd + 1) * 4, :]
                    src = p.t[:].rearrange("p (a b) -> p a b", a=4)
                    k.op("dve", lambda e, dst=dst, src=src: e.tensor_copy(dst, src), reads=[p], writes=[hTf])
                for kk in range(16):
                    k.op("pe", lambda e, kk=kk: e.matmul(plg.t[:], hTf.t[:, kk, :], wr.t[:, kk, :], start=(kk == 0), stop=(kk == 15)),
                         reads=[hTf, wr], writes=[plg])
                k.op("dve", lambda e: e.tensor_tensor(lg.t[:], plg.t[:], brb.t[:], ALU.add), reads=[plg, brb], writes=[lg])
                k.op("dve", lambda e: e.max(m8.t[:], lg.t[:]), reads=[lg], writes=[m8])
                k.op("dve", lambda e, qt=qt: e.tensor_scalar(maskf.t[:, qt, :], lg.t[:], m8.t[:, 3:4], None, op0=ALU.is_ge),
                     reads=[lg, m8], writes=[maskf])
                k.op("pool", lambda e, qt=qt: e.tensor_copy(maskb.t[:, qt, :], maskf.t[:, qt, :]), reads=[maskf], writes=[maskb])
                k.op("dve", lambda e: e.tensor_scalar(sm.t[:, 0:1], m8.t[:, 0:1], -1.0, None, op0=ALU.mult),
                     reads=[m8], writes=[sm])
                k.op("act", lambda e: e.activation(ex.t[:], lg.t[:], AF.Exp, bias=sm.t[:, 0:1], scale=1.0),
                     reads=[lg, sm], writes=[ex])
                k.op("dve", lambda e, qt=qt: e.tensor_tensor(ex.t[:], ex.t[:], maskf.t[:, qt, :], ALU.mult),
                     reads=[ex, maskf], writes=[ex])
                k.op("dve", lambda e: e.reduce_sum(sm.t[:, 1:2], ex.t[:], AX.X), reads=[ex], writes=[sm])
                k.op("dve", lambda e: e.reciprocal(sm.t[:, 2:3], sm.t[:, 1:2]), reads=[sm], writes=[sm])
                k.op("dve", lambda e, qt=qt: e.tensor_scalar(gate.t[:, qt, :], ex.t[:], sm.t[:, 2:3], None, op0=ALU.mult),
                     reads=[ex, sm], writes=[gate])
                k.op("pe", lambda e, qt=qt: e.matmul(ppos.t[:], tri.t[:], maskb.t[:, qt, :], start=True, stop=(qt == 0)),
                     reads=[tri, maskb], writes=[ppos])
                for i2 in range(qt):
                    k.op("pe", lambda e, i2=i2, qt=qt: e.matmul(ppos.t[:], onesb.t[:], maskb.t[:, i2, :], start=False,
                                                                stop=(i2 == qt - 1)), reads=[onesb, maskb], writes=[ppos])
                k.op("act", lambda e, qt=qt: e.copy(pos.t[:, qt, :], ppos.t[:]), reads=[ppos], writes=[pos])
        with k.scope():
            NST = CAP // 128
            Sm = k.sb("Smat", [128, 8, CAP], BF16)
            SGq = [k.sb("SGq%d" % i, [128, CAP], BF16) for i in range(2)]
            hk = [k.sb("hk%d" % i, [128, 8, 128], BF16) for i in range(2)]
            XeT = k.sb("XeT", [128, 16, CAP], BF16)
            actT = k.sb("actT", [128, 16, CAP], BF16)
            Yec = [k.sb("Yec%d" % i, [128, NST, 512], BF16) for i in range(2)]
            SGT = k.sb("SGT", [128, NST, NQ], BF16)
            b1t = [k.sb("b1t%d" % i, [128, 32], F32) for i in range(2)]
            wst = [k.sb("mwst%d" % i, [128, 16, 256], F32) for i in range(2)]
            wbf = [k.sb("mwbf%d" % i, [128, 16, 256], BF16) for i in range(2)]
            gl = [k.sb("gl%d" % i, [128, CAP], F32) for i in range(2)]
            ln_ = [k.sb("ln%d" % i, [128, CAP], F32) for i in range(2)]
            sg_ = [k.sb("sg%d" % i, [128, CAP], F32) for i in range(2)]
            xps = k.ps("xps", [128, CAP])
            pgl = [k.ps("pgl%d" % i, [128, 2, CAP]) for i in range(2)]
            py = k.ps("py", [128, 2, 256])
            psg = k.ps("psg", [128, 1024], BF16)
            psc = k.ps("psc", [128, 512])
            wc = [0]
            un = [0]
            hc = [0]

            def load_w(src_ap):
                i = wc[0] % 2
                wc[0] += 1
                ws, wb = wst[i], wbf[i]
                k.dma("sp", [(ws.t[:, 4 * j:4 * j + 4, :], src_ap[:, 4 * j:4 * j + 4, :]) for j in range(4)], writes=[ws])
                if wc[0] % 3 == 0:
                    k.op("dve", lambda e: e.tensor_copy(wb.t[:], ws.t[:]), reads=[ws], writes=[wb])
                else:
                    k.op("act", lambda e: e.copy(wb.t[:], ws.t[:]), reads=[ws], writes=[wb])
                return wb

            for ex_ in range(NE):
                b1 = b1t[ex_ % 2]
                k.dma("act", [(b1.t[:], I["b_exp1"][ex_].rearrange("(c p) -> p c", p=128))], writes=[b1],
                      allow_slow_non_contiguous=True)
                k.op("dve", lambda e, b1=b1: e.tensor_scalar(b1.t[:, 16:32], b1.t[:, 16:32], 1.0, None, op0=ALU.add),
                     reads=[b1], writes=[b1])
                for qt in range(8):
                    k.op("dve", lambda e, qt=qt: e.tensor_scalar(Sm.t[:, qt, :], iota.t[:], pos.t[:, qt, ex_:ex_ + 1],
                                                                 maskf.t[:, qt, ex_:ex_ + 1], op0=ALU.is_equal, op1=ALU.mult),
                         reads=[iota, pos, maskf], writes=[Sm])
                for fk in range(16):
                    h_ = hk[hc[0] % 2]
                    hc[0] += 1
                    k.dma("pool", [(h_.t[:], S["h2s"][fk])], reads=[self.SB["h2s"]], writes=[h_])
                    for qt in range(8):
                        k.op("pe", lambda e, h_=h_, qt=qt: e.matmul(xps.t[:], h_.t[:, qt, :], Sm.t[:, qt, :],
                                                                    start=(qt == 0), stop=(qt == 7)),
                             reads=[h_, Sm], writes=[xps])
                    k.op("act", lambda e, fk=fk: e.copy(XeT.t[:, fk, :], xps.t[:]), reads=[xps], writes=[XeT])
                for qt in range(8):
                    q_ = SGq[qt % 2]
                    k.op("dve", lambda e, qt=qt, q_=q_: e.tensor_scalar(q_.t[:], iota.t[:], pos.t[:, qt, ex_:ex_ + 1],
                                                                        gate.t[:, qt, ex_:ex_ + 1], op0=ALU.is_equal, op1=ALU.mult),
                         reads=[iota, pos, gate], writes=[q_])
                    hb = qt % 2
                    for s_ in range(NST):
                        k.op("pe", lambda e, s_=s_, q_=q_, hb=hb: e.transpose(
                            psg.t[:, hb * 512 + s_ * 128:hb * 512 + (s_ + 1) * 128], q_.t[:, s_ * 128:(s_ + 1) * 128], identb.t[:]),
                            reads=[q_, identb], writes=[psg])
                    k.op("act", lambda e, qt=qt, hb=hb: e.copy(
                        SGT.t[:, :, qt * 128:(qt + 1) * 128], psg.t[:, hb * 512:(hb + 1) * 512].rearrange("p (a b) -> p a b", a=NST)),
                        reads=[psg], writes=[SGT])
                for cc in range(8):
                    wg = load_w(I["w_exp1"][ex_][:, cc * 256:(cc + 1) * 256].rearrange("(k p) n -> p k n", p=128))
                    wl = load_w(I["w_exp1"][ex_][:, DFF + cc * 256:DFF + (cc + 1) * 256].rearrange("(k p) n -> p k n", p=128))
                    for u in range(2):
                        f = cc * 2 + u
                        b = un[0] % 2
                        un[0] += 1
                        pg = pgl[b]
                        for kk in range(16):
                            k.op("pe", lambda e, kk=kk, u=u, pg=pg, wg=wg: e.matmul(
                                pg.t[:, 0, :], wg.t[:, kk, u * 128:(u + 1) * 128], XeT.t[:, kk, :],
                                start=(kk == 0), stop=(kk == 15)), reads=[wg, XeT], writes=[pg])
                        for kk in range(16):
                            k.op("pe", lambda e, kk=kk, u=u, pg=pg, wl=wl: e.matmul(
                                pg.t[:, 1, :], wl.t[:, kk, u * 128:(u + 1) * 128], XeT.t[:, kk, :],
                                start=(kk == 0), stop=(kk == 15)), reads=[wl, XeT], writes=[pg])
                        k.op("dve", lambda e, b=b, pg=pg, f=f: e.tensor_scalar(gl[b].t[:], pg.t[:, 0, :], b1.t[:, f:f + 1], 7.0,
                                                                              op0=ALU.add, op1=ALU.min),
                             reads=[pg, b1], writes=[gl[b]])
                        k.op("dve", lambda e, b=b, pg=pg, f=f: e.tensor_scalar(ln_[b].t[:], pg.t[:, 1, :], b1.t[:, 16 + f:17 + f], -6.0,
                                                                              op0=ALU.add, op1=ALU.max),
                             reads=[pg, b1], writes=[ln_[b]])
                        k.op("act", lambda e, b=b: e.activation(sg_[b].t[:], gl[b].t[:], AF.Sigmoid, scale=1.702),
                             reads=[gl[b]], writes=[sg_[b]])
                        k.op("dve", lambda e, b=b: e.tensor_tensor(sg_[b].t[:], gl[b].t[:], sg_[b].t[:], ALU.mult),
                             reads=[gl[b], sg_[b]], writes=[sg_[b]])
                        k.op("dve", lambda e, b=b, f=f: e.scalar_tensor_tensor(actT.t[:, f, :], ln_[b].t[:], 8.0, sg_[b].t[:],
                                                                               ALU.min, ALU.mult),
                             reads=[sg_[b], ln_[b]], writes=[actT])
                for c in range(4):
                    ye = Yec[c % 2]
                    for half in range(2):
                        c0 = c * 512 + half * 256
                        w2 = load_w(I["w_exp2"][ex_][:, c0:c0 + 256].rearrange("(k p) n -> p k n", p=128))
                        for s_ in range(NST):
                            for fk in range(16):
                                k.op("pe", lambda e, fk=fk, s_=s_, w2=w2: e.matmul(
                                    py.t[:, s_ % 2, :], actT.t[:, fk, s_ * 128:(s_ + 1) * 128], w2.t[:, fk, :],
                                    start=(fk == 0), stop=(fk == 15)), reads=[actT, w2], writes=[py])
                            k.op("act", lambda e, s_=s_, half=half, ye=ye: e.copy(ye.t[:, s_, half * 256:(half + 1) * 256],
                                                                               py.t[:, s_ % 2, :]), reads=[py], writes=[ye])
                    for qt in range(8):
                        for s_ in range(NST):
                            k.op("pe", lambda e, s_=s_, qt=qt, ye=ye: e.matmul(
                                psc.t[:], SGT.t[:, s_, qt * 128:(qt + 1) * 128], ye.t[:, s_, :],
                                start=(s_ == 0), stop=(s_ == NST - 1)), reads=[SGT, ye], writes=[psc])
                        k.op("dve", lambda e, qt=qt, c=c: e.tensor_tensor(
                            xacc.t[:, qt, c * 512:(c + 1) * 512], xacc.t[:, qt, c * 512:(c + 1) * 512], psc.t[:], ALU.add),
                            reads=[psc, xab[qt]], writes=[xab[qt]])
        with k.scope():
            identf = self.load_const("identf")
            b2all = k.sb("b2all", [NE, D], F32)
            k.dma("sp", [(b2all.t[:], I["b_exp2"])], writes=[b2all])
            g2b = self.bvec(5, "g2b")
            gT = [k.sb("gT%d" % i, [NE, 128], F32) for i in range(2)]
            pgt = k.ps("pgt", [NE, 128])
            pb2 = [k.ps("pb2_%d" % i, [128, 512]) for i in range(2)]
            xt = [k.sb("fxt%d" % i, [128, D], F32) for i in range(2)]
            x2 = [k.sb("fx2_%d" % i, [128, D], F32) for i in range(2)]
            n = 0
            if self.last:
                nfb = k.sb("nfb", [128, D], F32)
                k.dma("sp", [(nfb.t[:], I["norm_final"].partition_broadcast(128))], writes=[nfb])
                junk = k.sb("fjunk", [128, D], BF16)
                ms = [k.sb("fms%d" % i, [128, 4], F32) for i in range(2)]
                yo = [k.sb("fyo%d" % i, [128, D], F32) for i in range(2)]
            for qt in range(8):
                g_ = gT[qt % 2]
                k.op("pe", lambda e, qt=qt: e.transpose(pgt.t[:], gate.t[:, qt, :], identf.t[:]), reads=[gate, identf], writes=[pgt])
                k.op("act", lambda e, g_=g_: e.copy(g_.t[:], pgt.t[:]), reads=[pgt], writes=[g_])
                x_, x2_ = xt[qt % 2], x2[qt % 2]
                k.dma("sp", [(x_.t[:], S["x1"][qt * 128:(qt + 1) * 128, :])], reads=[self.SB["x1"]], writes=[x_])
                for c in range(4):
                    p = pb2[n % 2]
                    n += 1
                    k.op("pe", lambda e, p=p, c=c, g_=g_: e.matmul(p.t[:], g_.t[:], b2all.t[:, c * 512:(c + 1) * 512],
                                                                   start=True, stop=True), reads=[g_, b2all], writes=[p])
                    k.op("dve", lambda e, p=p, qt=qt, c=c: e.tensor_tensor(
                        xacc.t[:, qt, c * 512:(c + 1) * 512], xacc.t[:, qt, c * 512:(c + 1) * 512], p.t[:], ALU.add),
                        reads=[p, xab[qt]], writes=[xab[qt]])
                k.op("pool", lambda e, qt=qt: e.tensor_tensor(xacc.t[:, qt, :], xacc.t[:, qt, :], g2b.t[:], ALU.mult),
                     reads=[xab[qt], g2b], writes=[xab[qt]])
                k.op("dve", lambda e, qt=qt, x_=x_, x2_=x2_: e.tensor_tensor(x2_.t[:], xacc.t[:, qt, :], x_.t[:], ALU.add),
                     reads=[xab[qt], x_], writes=[x2_])
                if self.last:
                    m_, y_ = ms[qt % 2], yo[qt % 2]
                    k.op("pool", lambda e, m_=m_: e.memset(m_.t[:, 0:1], 0.0), writes=[m_])
                    k.op("act", lambda e, m_=m_, x2_=x2_: e.activation(junk.t[:], x2_.t[:], AF.Square, scale=float(D ** -0.5),
                                                                       accum_out=m_.t[:, 0:1]), reads=[x2_, m_], writes=[junk, m_])
                    k.op("act", lambda e, m_=m_: e.activation(m_.t[:, 1:2], m_.t[:, 0:1], AF.Sqrt, bias=EPS, scale=1.0),
                         reads=[m_], writes=[m_])
                    k.op("dve", lambda e, m_=m_: e.reciprocal(m_.t[:, 2:3], m_.t[:, 1:2]), reads=[m_], writes=[m_])
                    k.op("dve", lambda e, m_=m_, y_=y_, x2_=x2_: e.scalar_tensor_tensor(
                        y_.t[:], x2_.t[:], m_.t[:, 2:3], nfb.t[:], ALU.mult, ALU.mult), reads=[x2_, m_, nfb], writes=[y_])
                    k.dma("act", [(self.out[qt * 128:(qt + 1) * 128, :], y_.t[:])], reads=[y_], writes=[self.outB])
                else:
                    k.dma("act", [(self.out[qt * 128:(qt + 1) * 128, :], x2_.t[:])], reads=[x2_], writes=[self.outB])


_PROG_CACHE = {}


def _get_prog(last):
    if last not in _PROG_CACHE:
        p = Prog(last)
        p.build()
        _PROG_CACHE[last] = p
    return _PROG_CACHE[last]


def kernel(**inputs):
    x = np.ascontiguousarray(np.asarray(inputs["x"], dtype=np.float32))
    B = x.shape[0]
    sc = shared_consts()
    ccs = [core_consts(r) for r in range(4)]
    for layer in range(2):
        prog = _get_prog(layer == 1)
        maps = []
        for c in range(8):
            b, r = c // 4, c % 4
            pad = 1024 * (3 - r)
            m = {}
            for n in prog.I.keys():
                if n == "xin":
                    xl = np.zeros((NTOK, D), np.float32)
                    xl[pad:TL] = x[b, :TL - pad]
                    xl[TL:] = x[b, 1024 * r:1024 * (r + 1)]
                    m[n] = xl
                elif n == "cvec":
                    m[n] = np.ascontiguousarray(np.asarray(inputs["c"], dtype=np.float32)[b])
                elif n == "norm_final":
                    m[n] = np.ascontiguousarray(np.asarray(inputs[n], dtype=np.float32))
                elif n in WEIGHT_SPECS:
                    m[n] = np.ascontiguousarray(np.asarray(inputs[n], dtype=np.float32)[layer])
                elif n in sc:
                    m[n] = sc[n]
                else:
                    m[n] = ccs[r][n]
            maps.append(m)
        res = run_bass_kernel_spmd(prog.nc, maps, core_ids=list(range(8)))
        xn = np.empty_like(x)
        for c in range(8):
            b, r = c // 4, c % 4
            xn[b, 1024 * r:1024 * (r + 1)] = res.results[c]["out"]
        x = xn
    return x
```
